# Optimizing a Trainium2 kernel written in Bass

```python
import jax, jax.numpy as jnp
from jax import lax
import numpy as np

D_MODEL = 2048
BATCH = 2
SEQ = 8192
DEPTH = 4

CTX_LEN = 256
GRID_W = 64
RWKV_WIDTH = 1024
RWKV_HEAD = 64
RWKV_HEADS = RWKV_WIDTH // RWKV_HEAD
DECAY_LORA = 64
AAA_LORA = 64
GATE_LORA = 128
GN_EPS = 64e-5
CONV_WIDTH = 1024
CONV_K = 31
LN_EPS = 1e-5
N_EXPERTS = 16
EXPERT_FF = 1024
EC_CAPACITY = 2
N_MOD = 6
RMS_EPS = 1e-6

SHIFT_COLS = 3 * RWKV_WIDTH + DECAY_LORA + AAA_LORA
OFF_WA_F = 3 * RWKV_WIDTH
OFF_WA_B = OFF_WA_F + DECAY_LORA + AAA_LORA
OFF_GLAT = OFF_WA_B + DECAY_LORA + AAA_LORA
RWKV_SCAN_COLS = OFF_GLAT
OFF_GLU = OFF_GLAT + GATE_LORA
OFF_GATE = OFF_GLU + 2 * CONV_WIDTH
IN_COLS = OFF_GATE + 2 * D_MODEL

kernel_name = 'hybrid_rwkv7_conformer_ecmoe_dit'


def _rmsnorm(x, g):
    xf = x.astype(jnp.float32)
    y = xf * lax.rsqrt(jnp.mean(xf * xf, axis=-1, keepdims=True) + RMS_EPS)
    return (y * g.astype(jnp.float32)).astype(x.dtype)


def _modulate(h, shift, scale):
    return h * (1 + scale) + shift


def _heads(t):
    return t.reshape(t.shape[:-1] + (RWKV_HEADS, RWKV_HEAD))


def _token_shift(z, direction):
    if direction > 0:
        return jnp.pad(z[:, :-1], ((0, 0), (1, 0), (0, 0)))
    return jnp.pad(z[:, 1:], ((0, 0), (0, 1), (0, 0)))


def _rwkv_direction(P, lp, d):
    direction = 1 if d == 0 else -1
    off = OFF_WA_F if d == 0 else OFF_WA_B
    z = jnp.concatenate([P[..., :3 * RWKV_WIDTH], P[..., off:off + DECAY_LORA + AAA_LORA]], axis=-1)
    z = z + lp['tok_mu'][d] * (_token_shift(z, direction) - z)
    r, k, v, zw, za = jnp.split(z, [RWKV_WIDTH, 2 * RWKV_WIDTH, 3 * RWKV_WIDTH, 3 * RWKV_WIDTH + DECAY_LORA], axis=-1)
    w_log = -jax.nn.softplus(-(lp['w0'][d] + jnp.tanh(zw) @ lp['w2'][d])) - 0.5
    decay = jnp.exp(-jnp.exp(w_log.astype(jnp.float32)))
    a = jax.nn.sigmoid(lp['a0'][d] + za @ lp['a2'][d])
    kk = _heads((k * lp['k_k'][d]).astype(jnp.float32))
    kk = kk / jnp.maximum(jnp.sqrt(jnp.sum(kk * kk, axis=-1, keepdims=True)), 1e-12)
    k = k * (1 + (a - 1) * lp['k_a'][d])
    return _heads(r), _heads(k), _heads(v), _heads(decay), kk, _heads(a)


def _delta_scan(r, k, v, decay, kk, a, s0, reverse, emit=True):
    xs = tuple(jnp.moveaxis(t.astype(jnp.float32), 1, 0) for t in (r, k, v, decay, kk, a))

    def step(S, inp):
        r_t, k_t, v_t, w_t, kk_t, a_t = inp
        sa = jnp.einsum('bhvk,bhk->bhv', S, kk_t)
        S = (S * w_t[:, :, None, :] - sa[..., None] * (kk_t * a_t)[:, :, None, :]
             + v_t[..., None] * k_t[:, :, None, :])
        y = jnp.einsum('bhvk,bhk->bhv', S, r_t) if emit else None
        return S, y

    s_T, ys = lax.scan(step, s0, xs, reverse=reverse)
    return (jnp.moveaxis(ys, 0, 1) if emit else None), s_T


def _rwkv_branch(P, lp, s0_f, s0_b):
    outs = []
    for d, s0 in ((0, s0_f), (1, s0_b)):
        r, k, v, w, kk, a = _rwkv_direction(P, lp, d)
        y, s_T = _delta_scan(r, k, v, w, kk, a, s0, reverse=(d == 1))
        bonus = jnp.sum(r * k * _heads(lp['r_k']), axis=-1, keepdims=True) * v
        outs.append((y, bonus, s_T))
    (y_f, bonus_f, sT_f), (y_b, bonus_b, sT_b) = outs
    y = y_f + y_b
    mu = jnp.mean(y, axis=-1, keepdims=True)
    var = jnp.mean(jnp.square(y - mu), axis=-1, keepdims=True)
    y = (y - mu) * lax.rsqrt(var + GN_EPS) * _heads(lp['lnx_g']) + _heads(lp['lnx_b'])
    y = y.astype(P.dtype) + bonus_f + bonus_b
    B, L = P.shape[0], P.shape[1]
    g = jax.nn.sigmoid(P[..., OFF_GLAT:OFF_GLU]) @ lp['g2']
    return (y.reshape(B, L, RWKV_WIDTH) * g) @ lp['w_rwkv_out'], sT_f, sT_b


def _conformer_branch(P, lp, grid):
    glu = P[..., OFF_GLU:OFF_GATE]
    u = glu[..., :CONV_WIDTH] * jax.nn.sigmoid(glu[..., CONV_WIDTH:])
    B, L, C = u.shape
    if grid:
        rows = L // GRID_W
        seqs = u.reshape(B * rows, GRID_W, C)
    else:
        seqs = u
    pad = CONV_K // 2
    seqs = lax.conv_general_dilated(seqs, lp['conv_w'][:, None, :], window_strides=(1,),
                                    padding=[(pad, pad)], dimension_numbers=('NWC', 'WIO', 'NWC'),
                                    feature_group_count=C)
    u = seqs.reshape(B, L, C) + lp['conv_b']
    uf = u.astype(jnp.float32)
    mu = jnp.mean(uf, axis=-1, keepdims=True)
    var = jnp.mean(jnp.square(uf - mu), axis=-1, keepdims=True)
    un = (uf - mu) * lax.rsqrt(var + LN_EPS) * lp['conv_ln_g'] + lp['conv_ln_b']
    return jax.nn.silu(un).astype(P.dtype) @ lp['w_conv_out']


def _mixer_sublayer(x, shift, scale, gate, lp, s0_f, s0_b, grid):
    h = _modulate(_rmsnorm(x, lp['norm1_g']), shift, scale)
    P = h @ lp['w_in']
    b_r, sT_f, sT_b = _rwkv_branch(P, lp, s0_f, s0_b)
    b_c = _conformer_branch(P, lp, grid)
    g_r = jax.nn.sigmoid(P[..., OFF_GATE:OFF_GATE + D_MODEL])
    g_c = jax.nn.sigmoid(P[..., OFF_GATE + D_MODEL:])
    m = (g_r * b_r + g_c * b_c) @ lp['w_o']
    return x + gate * m, sT_f, sT_b


def _context_states(ctx, shift, scale, lp, s0):
    h = _modulate(_rmsnorm(ctx, lp['norm1_g']), shift, scale)
    P = h @ lp['w_in'][:, :RWKV_SCAN_COLS]
    states = []
    for d in range(2):
        r, k, v, w, kk, a = _rwkv_direction(P, lp, d)
        _, s_T = _delta_scan(r, k, v, w, kk, a, s0, reverse=(d == 1), emit=False)
        states.append(s_T)
    return states[0], states[1]


def _ec_moe(h, lp):
    B, L, D = h.shape
    cap = EC_CAPACITY * L // N_EXPERTS
    aff = jax.nn.softmax((h @ lp['w_router']).astype(jnp.float32), axis=-1)
    vals, idx = lax.top_k(jnp.swapaxes(aff, 1, 2), cap)
    xs = jax.vmap(lambda hb, ib: hb[ib])(h, idx)
    gh = jnp.einsum('becd,edf->becf', xs, lp['w_exp_gate'])
    uh = jnp.einsum('becd,edf->becf', xs, lp['w_exp_up'])
    y = jnp.einsum('becf,efd->becd', jax.nn.silu(gh) * uh, lp['w_exp_down'])
    y = y * vals[..., None].astype(h.dtype)
    return jax.vmap(lambda ib, yb: jnp.zeros((L, D), h.dtype).at[ib.reshape(-1)].add(yb.reshape(-1, D)))(idx, y)


def setup_inputs(seed: int = 0) -> dict:
    key = jax.random.key(seed)
    ks = iter(jax.random.split(key, 48))

    def nrm(shape, scale):
        return scale * jax.random.normal(next(ks), shape, jnp.float32)

    NL, D, R, CC = DEPTH, D_MODEL, RWKV_WIDTH, CONV_WIDTH
    return {
        'x': nrm((BATCH, SEQ, D), 1.0),
        'c': nrm((BATCH, D), 1.0),
        'ctx': nrm((BATCH, CTX_LEN, D), 1.0),
        'c_ctx': nrm((D,), 1.0),
        'w_mod': nrm((NL, D, N_MOD * D), 0.5 * D ** -0.5),
        'b_mod': nrm((NL, N_MOD * D), 0.02),
        'norm1_g': 1.0 + nrm((NL, D), 0.02),
        'w_in': nrm((NL, D, IN_COLS), D ** -0.5),
        'tok_mu': jax.random.uniform(next(ks), (NL, 2, SHIFT_COLS), jnp.float32),
        'w0': -2.5 + nrm((NL, 2, R), 1.5),
        'w2': nrm((NL, 2, DECAY_LORA, R), DECAY_LORA ** -0.5),
        'a0': nrm((NL, 2, R), 0.5),
        'a2': nrm((NL, 2, AAA_LORA, R), AAA_LORA ** -0.5),
        'k_k': 0.85 + nrm((NL, 2, R), 0.05),
        'k_a': 1.0 + nrm((NL, 2, R), 0.05),
        'r_k': nrm((NL, R), 0.1),
        'g2': nrm((NL, GATE_LORA, R), GATE_LORA ** -0.5),
        'lnx_g': 1.0 + nrm((NL, R), 0.02),
        'lnx_b': nrm((NL, R), 0.02),
        'w_rwkv_out': nrm((NL, R, D), R ** -0.5),
        'conv_w': nrm((NL, CONV_K, CC), CONV_K ** -0.5),
        'conv_b': nrm((NL, CC), 0.02),
        'conv_ln_g': 1.0 + nrm((NL, CC), 0.02),
        'conv_ln_b': nrm((NL, CC), 0.02),
        'w_conv_out': nrm((NL, CC, D), CC ** -0.5),
        'w_o': nrm((NL, D, D), D ** -0.5),
        'norm2_g': 1.0 + nrm((NL, D), 0.02),
        'w_router': nrm((NL, D, N_EXPERTS), D ** -0.5),
        'w_exp_gate': nrm((NL, N_EXPERTS, D, EXPERT_FF), D ** -0.5),
        'w_exp_up': nrm((NL, N_EXPERTS, D, EXPERT_FF), D ** -0.5),
        'w_exp_down': nrm((NL, N_EXPERTS, EXPERT_FF, D), EXPERT_FF ** -0.5),
        'final_g': 1.0 + nrm((D,), 0.02),
    }


def reference(x, c, ctx, c_ctx, w_mod, b_mod, norm1_g, w_in, tok_mu, w0, w2, a0, a2, k_k, k_a, r_k,
              g2, lnx_g, lnx_b, w_rwkv_out, conv_w, conv_b, conv_ln_g, conv_ln_b, w_conv_out, w_o,
              norm2_g, w_router, w_exp_gate, w_exp_up, w_exp_down, final_g):
    batch = x.shape[0]
    zero_state = jnp.zeros((batch, RWKV_HEADS, RWKV_HEAD, RWKV_HEAD), jnp.float32)
    for l in range(DEPTH):
        lp = dict(norm1_g=norm1_g[l], w_in=w_in[l], tok_mu=tok_mu[l], w0=w0[l], w2=w2[l], a0=a0[l],
                  a2=a2[l], k_k=k_k[l], k_a=k_a[l], r_k=r_k[l], g2=g2[l], lnx_g=lnx_g[l], lnx_b=lnx_b[l],
                  w_rwkv_out=w_rwkv_out[l], conv_w=conv_w[l], conv_b=conv_b[l], conv_ln_g=conv_ln_g[l],
                  conv_ln_b=conv_ln_b[l], w_conv_out=w_conv_out[l], w_o=w_o[l], norm2_g=norm2_g[l],
                  w_router=w_router[l], w_exp_gate=w_exp_gate[l], w_exp_up=w_exp_up[l],
                  w_exp_down=w_exp_down[l])
        m_lat = jnp.split((jax.nn.silu(c) @ w_mod[l] + b_mod[l])[:, None, :], N_MOD, axis=-1)
        m_ctx = jnp.split((jax.nn.silu(c_ctx) @ w_mod[l] + b_mod[l])[None, None, :], N_MOD, axis=-1)
        last = l == DEPTH - 1
        if not last:
            ctx_mid, s_f, s_b = _mixer_sublayer(ctx, m_ctx[0], m_ctx[1], m_ctx[2], lp,
                                                zero_state, zero_state, grid=False)
        else:
            s_f, s_b = _context_states(ctx, m_ctx[0], m_ctx[1], lp, zero_state)
        x, _, _ = _mixer_sublayer(x, m_lat[0], m_lat[1], m_lat[2], lp, s_f, s_b, grid=True)
        x = x + m_lat[5] * _ec_moe(_modulate(_rmsnorm(x, lp['norm2_g']), m_lat[3], m_lat[4]), lp)
        if not last:
            ctx = ctx_mid + m_ctx[5] * _ec_moe(
                _modulate(_rmsnorm(ctx_mid, lp['norm2_g']), m_ctx[3], m_ctx[4]), lp)
    return _rmsnorm(x, final_g)
```

```python
import contextlib
import numpy as np
import concourse.bass as bass
import concourse.mybir as mybir
from concourse.bass_utils import run_bass_kernel_spmd

F32 = mybir.dt.float32
BF16 = mybir.dt.bfloat16
I32 = mybir.dt.int32
U32 = mybir.dt.uint32
AF = mybir.ActivationFunctionType
ALU = mybir.AluOpType
AX = mybir.AxisListType


import os
SAME_ENG_WAIT = os.environ.get('SAME_ENG_WAIT', '1') == '1'


class Cfg:
    def __init__(s, D=2048, L=8192, CTX=256, R=1024, CW=1024, E=16, FF=1024, NL=4, GW=64):
        s.D, s.L, s.CTX, s.R, s.CW, s.E, s.FF, s.NL, s.GW = D, L, CTX, R, CW, E, FF, NL, GW
        s.HD = 64
        s.NH = R // 64
        s.DL, s.AL, s.GL = 64, 64, 128
        s.CK = 31
        s.NT = CTX + L
        s.OFF_WA_F = 3 * R
        s.OFF_WA_B = s.OFF_WA_F + 128
        s.OFF_GLAT = s.OFF_WA_B + 128
        s.OFF_GLU = s.OFF_GLAT + s.GL
        s.OFF_GATE = s.OFF_GLU + 2 * CW
        s.INC = s.OFF_GATE + 2 * D
        s.CAP_L = 2 * L // E
        s.CAP_C = 2 * CTX // E


class View:
    __slots__ = ("t", "ap")

    def __init__(s, t, ap):
        s.t, s.ap = t, ap

    def __getitem__(s, k):
        return View(s.t, s.ap[k])

    def bc(s, shape):
        return View(s.t, s.ap.to_broadcast(list(shape)))

    def re(s, pat, **kw):
        return View(s.t, s.ap.rearrange(pat, **kw))

    def bc3(s, n):
        sh = list(s.ap.shape)
        return View(s.t, s.ap.unsqueeze(1).to_broadcast([sh[0], n, sh[1]]))


class Tile:
    __slots__ = ("ap", "w", "r", "name")

    def __init__(s, ap, name):
        s.ap, s.name = ap, name
        s.w = None
        s.r = {}

    def __getitem__(s, k):
        return View(s, s.ap[k])


class Builder:
    ENG = ("pe", "act", "dve", "pool", "sp")

    def __init__(s, nc, ndma=8):
        s.nc = nc
        s.h = {"pe": nc.tensor, "act": nc.scalar, "dve": nc.vector, "pool": nc.gpsimd, "sp": nc.sync}
        s.prog = {e: [] for e in s.ENG}
        s.sem = {e: nc.alloc_semaphore(name="s_" + e) for e in s.ENG}
        s.cnt = {e: 0 for e in s.ENG}
        s.waited = {e: {} for e in s.ENG}
        s.dq = {}
        for q in ("sp", "pool", "act"):
            s.dq[q] = dict(sems=[nc.alloc_semaphore(name=f"d_{q}{i}") for i in range(ndma)], n=0)
        s.stack = contextlib.ExitStack()
        s.stage_stack = None
        s.uid = 0

    def _alloc(s, kind, name, shape, dt, stage=True):
        s.uid += 1
        nm = f"{name}_{s.uid}"
        st = s.stage_stack if (stage and s.stage_stack is not None) else s.stack
        if kind == "sb":
            t = st.enter_context(s.nc.sbuf_tensor(nm, list(shape), dt))
        else:
            t = st.enter_context(s.nc.psum_tensor(nm, list(shape), dt))
        return Tile(t, nm)

    def sb(s, name, shape, dt, stage=True):
        return s._alloc("sb", name, shape, dt, stage)

    def ps(s, name, shape, dt=F32, stage=True):
        return s._alloc("ps", name, shape, dt, stage)

    def pool_of(s, n, name, shape, dt, kind="sb"):
        return Ring([s._alloc(kind, name, shape, dt) for _ in range(n)])

    @contextlib.contextmanager
    def stage(s):
        s.barrier()
        old = s.stage_stack
        s.stage_stack = contextlib.ExitStack()
        try:
            yield
        finally:
            s.barrier()
            s.stage_stack.close()
            s.stage_stack = old

    def _wait(s, eng, tok):
        if tok is None:
            return
        sem, val = tok
        key = id(sem)
        if (not SAME_ENG_WAIT) and sem is s.sem.get(eng):
            return
        if s.waited[eng].get(key, 0) >= val:
            return
        s.waited[eng][key] = val
        s.h[eng].wait_ge(sem, val)

    def _deps(s, eng, reads, writes, own_tok_is_pe=False):
        for t in reads:
            s._wait(eng, t.w)
        for t in writes:
            if not (own_tok_is_pe and t.w is not None and t.w[0] is s.sem["pe"]):
                s._wait(eng, t.w)
            for tok in t.r.values():
                s._wait(eng, tok)

    def _mark(s, tok, reads, writes):
        for t in reads:
            t.r[id(tok[0])] = tok
        for t in writes:
            t.w = tok
            t.r = {}

    def op(s, eng, fn, reads=(), writes=()):
        s._deps(eng, reads, writes, own_tok_is_pe=(eng == "pe"))
        s.cnt[eng] += 1
        tok = (s.sem[eng], s.cnt[eng])
        sem = s.sem[eng]
        fn(s.h[eng]).then_inc(sem, 1)
        s._mark(tok, reads, writes)
        return tok

    def dma(s, q, out, in_, reads=(), writes=(), indirect=None, **kw):
        dq = s.dq[q]
        i = dq["n"] % len(dq["sems"])
        gen = dq["n"] // len(dq["sems"])
        dq["n"] += 1
        sem = dq["sems"][i]
        s._wait(q, (sem, 16 * gen))
        s._deps(q, reads, writes)
        tok = (sem, 16 * (gen + 1))
        if indirect is None:
            s.h[q].dma_start(out=out, in_=in_, **kw).then_inc(sem, 16)
        else:
            indirect(s.h[q]).then_inc(sem, 16)
        s._mark(tok, reads, writes)
        return tok

    def barrier(s):
        toks = [(s.sem[e], s.cnt[e]) for e in s.ENG if s.cnt[e] > 0]
        for q, dq in s.dq.items():
            ns = len(dq["sems"])
            for i, sem in enumerate(dq["sems"]):
                used = (dq["n"] - i + ns - 1) // ns
                if used > 0:
                    toks.append((sem, 16 * used))
        for e in s.ENG:
            for tok in toks:
                if tok[0] is s.sem[e]:
                    continue
                s._wait(e, tok)

    def finish(s):
        s.barrier()
        s.stack.close()


class Ring:
    def __init__(s, tiles):
        s.t, s.i = tiles, 0

    def next(s):
        t = s.t[s.i % len(s.t)]
        s.i += 1
        return t


def _tiles(*vs):
    return [v.t for v in vs if isinstance(v, View)]


def _ap(v):
    return v.ap if isinstance(v, View) else v


def tt(g, eng, out, a, b, op):
    g.op(eng, lambda h: h.tensor_tensor(out=out.ap, in0=a.ap, in1=b.ap, op=op), _tiles(a, b), _tiles(out))


def ts(g, eng, out, a, s1, op0, s2=None, op1=None):
    kw = dict(out=out.ap, in0=a.ap, scalar1=_ap(s1), scalar2=_ap(s2), op0=op0)
    if op1 is not None:
        kw["op1"] = op1
    g.op(eng, lambda h: h.tensor_scalar(**kw), _tiles(a, s1, s2), _tiles(out))


def stt(g, eng, out, a, sc, b, op0, op1):
    g.op(eng, lambda h: h.scalar_tensor_tensor(out=out.ap, in0=a.ap, scalar=_ap(sc), in1=b.ap, op0=op0, op1=op1),
         _tiles(a, sc, b), _tiles(out))


def act(g, out, a, func, bias=None, scale=None):
    kw = dict(out=out.ap, in_=a.ap, func=func)
    if bias is not None:
        kw["bias"] = _ap(bias)
    if scale is not None:
        kw["scale"] = _ap(scale)
    g.op("act", lambda h: h.activation(**kw), _tiles(a, bias, scale), _tiles(out))


def mm(g, out, lhsT, rhs, start=True, stop=True):
    g.op("pe", lambda h: h.matmul(out.ap, lhsT.ap, rhs.ap, start=start, stop=stop), _tiles(lhsT, rhs), _tiles(out))


def tr(g, out, a, ident):
    g.op("pe", lambda h: h.transpose(out.ap, a.ap, ident.ap), _tiles(a, ident), _tiles(out))


def cp(g, eng, out, a):
    if eng == "act":
        g.op("act", lambda h: h.copy(out=out.ap, in_=a.ap), _tiles(a), _tiles(out))
    else:
        g.op(eng, lambda h: h.tensor_copy(out=out.ap, in_=a.ap), _tiles(a), _tiles(out))


def recip(g, out, a):
    g.op("dve", lambda h: h.reciprocal(out=out.ap, in_=a.ap), _tiles(a), _tiles(out))


def mset(g, eng, out, val):
    g.op(eng, lambda h: h.memset(out.ap, val), (), _tiles(out))


def ld(g, q, out, dram, **kw):
    g.dma(q, out.ap, dram, reads=(), writes=[out.t], **kw)


def st(g, q, dram, a, **kw):
    g.dma(q, dram, a.ap, reads=[a.t], writes=(), **kw)


class Alt:
    def __init__(s, engs):
        s.e, s.i = engs, 0

    def __call__(s):
        s.i += 1
        return s.e[s.i % len(s.e)]


def linear_fm(g, XT, K, W, N, YT, ydt, tiles, func=AF.Identity, GN=512):
    linear_fm_multi(g, [(XT, K, W, N, YT, tiles, func)], ydt, GN=GN)


def linear_fm_multi(g, jobs, ydt, GN=512):
    KCm = max(j[1] for j in jobs) // 128
    GN = min(GN, max(j[3] for j in jobs))
    with g.stage():
        wbr = g.pool_of(2, "lw", [128, KCm, GN], BF16)
        wsr = g.pool_of(3, "lws", [128, GN], F32)
        xr = g.pool_of(2, "lx", [128, KCm, 512], BF16)
        yr = g.pool_of(2, "ly", [128, GN // 128, 512], ydt)
        psr = g.pool_of(4, "lps", [128, 512], F32, kind="ps")
        ce = Alt(["dve", "pool"])
        ee = Alt(["act", "dve"])

        def load_w(grp):
            (XT, K, W, N, YT, tiles, func), n0 = grp
            gn = min(GN, N - n0)
            wb = wbr.next()
            for kc in range(K // 128):
                ws = wsr.next()
                ld(g, "act", ws[:, :gn], W[kc * 128:(kc + 1) * 128, n0:n0 + gn])
                cp(g, ce(), wb[:, kc, :gn], ws[:, :gn])
            return wb
        groups = [(j, n0) for j in jobs for n0 in range(0, j[3], GN)]
        wb_next = load_w(groups[0])
        for gi, grp in enumerate(groups):
            (XT, K, W, N, YT, tiles, func), n0 = grp
            KC = K // 128
            gn = min(GN, N - n0)
            wb = wb_next
            if gi + 1 < len(groups):
                wb_next = load_w(groups[gi + 1])
            for (t0, tsz) in tiles:
                xt = xr.next()
                ld(g, "sp", xt[:, :KC, :tsz], XT[:, t0:t0 + tsz].rearrange("(kc p) t -> p kc t", p=128))
                yo = yr.next()
                for m in range(gn // 128):
                    ps = psr.next()
                    for kc in range(KC):
                        mm(g, ps[:, :tsz], wb[:, kc, m * 128:(m + 1) * 128], xt[:, kc, :tsz], kc == 0, kc == KC - 1)
                    if func == AF.Identity and ee() == "dve":
                        cp(g, "dve", yo[:, m, :tsz], ps[:, :tsz])
                    else:
                        act(g, yo[:, m, :tsz], ps[:, :tsz], func)
                st(g, "pool", YT[n0:n0 + gn, t0:t0 + tsz].rearrange("(m p) t -> p m t", p=128), yo[:, :gn // 128, :tsz])


def transpose_dram(g, src, A, B, dst, dt, ident):
    with g.stage():
        sr = g.pool_of(2, "ts", [128, B], dt)
        orr = g.pool_of(3, "to", [128, 4, 128], dt)
        psr = g.pool_of(3, "tp", [128, 4, 128], dt, kind="ps")
        ee = Alt(["act", "dve"])
        for a0 in range(0, A, 128):
            s_ = sr.next()
            ld(g, "sp", s_[:, :], src[a0:a0 + 128, :])
            for b0 in range(0, B, 512):
                nb = min(4, (B - b0) // 128)
                ps = psr.next()
                for i in range(nb):
                    tr(g, ps[:, i, :], s_[:, b0 + i * 128:b0 + (i + 1) * 128], ident)
                ot = orr.next()
                cp(g, ee(), ot[:, :nb, :], ps[:, :nb, :])
                st(g, "pool", dst[b0:b0 + nb * 128, a0:a0 + 128].rearrange("(i p) a -> p i a", p=128), ot[:, :nb, :])


def vec_layout(c):
    KD, KR, KC = c.D // 128, c.R // 128, c.CW // 128
    off, o = {}, 0

    def add(n, w):
        nonlocal o
        off[n] = o
        o += w
    add("n1g", KD); add("n2g", KD); add("fin", KD); add("bmod", 6 * KD)
    for d in range(2):
        for n in ("mu_r", "mu_k", "mu_v", "w0", "a0", "kk", "ka"):
            add(f"{n}{d}", KR)
        add(f"mu_wa{d}", 1)
    for n in ("rk", "lng", "lnb"):
        add(n, KR)
    add("convw", KC * 31); add("convb", KC); add("clg", KC); add("clb", KC)
    return off, o


def fm(a):
    n = a.shape[-1] // 128
    return np.swapaxes(a.reshape(a.shape[:-1] + (n, 128)), -1, -2)


def pack_vecs(c, I):
    off, nv = vec_layout(c)
    R = c.R
    V = np.zeros((c.NL, 128, nv), np.float32)

    def put(n, a):
        V[:, :, off[n]:off[n] + a.shape[-1]] = a
    put("n1g", fm(I["norm1_g"])); put("n2g", fm(I["norm2_g"]))
    put("fin", np.broadcast_to(fm(I["final_g"])[None], (c.NL, 128, c.D // 128)))
    bm = I["b_mod"].reshape(c.NL, 6, c.D)
    put("bmod", np.concatenate([fm(bm[:, j]) for j in range(6)], axis=-1))
    for d in range(2):
        mu = I["tok_mu"][:, d]
        put(f"mu_r{d}", fm(mu[:, 0:R])); put(f"mu_k{d}", fm(mu[:, R:2 * R])); put(f"mu_v{d}", fm(mu[:, 2 * R:3 * R]))
        put(f"mu_wa{d}", mu[:, 3 * R:3 * R + 128][:, :, None])
        put(f"w0{d}", fm(I["w0"][:, d])); put(f"a0{d}", fm(I["a0"][:, d]))
        put(f"kk{d}", fm(I["k_k"][:, d])); put(f"ka{d}", fm(I["k_a"][:, d]))
    put("rk", fm(I["r_k"])); put("lng", fm(I["lnx_g"])); put("lnb", fm(I["lnx_b"]))
    cw = np.transpose(I["conv_w"], (0, 2, 1))
    KC = c.CW // 128
    cw = cw.reshape(c.NL, KC, 128, 31).transpose(0, 2, 1, 3).reshape(c.NL, 128, KC * 31)
    put("convw", cw)
    put("convb", fm(I["conv_b"])); put("clg", fm(I["conv_ln_g"])); put("clb", fm(I["conv_ln_b"]))
    return V


def make_consts():
    C = np.zeros((128, 6, 128), np.float32)
    C[:, 0, :] = np.eye(128)
    C[:, 1, :] = 1.0
    C[:64, 2, :64] = 1.0
    C[64:, 2, 64:] = 1.0
    s = np.arange(128)[:, None] % 64
    t = np.arange(128)[None, :] % 64
    q = (np.arange(128)[None, :] < 64)
    C[:, 3, :] = np.where(q, s < t, s <= t)
    C[:, 4, :] = np.where(q, s > t, s >= t)
    return C


C0 = float(np.exp(-0.5))
WNAMES = ["w_mod", "w_in", "w2a2", "g2", "w_rwkv_out", "w_conv_out", "w_o", "w_router", "w_exp_gate", "w_exp_up", "w_exp_down"]


def build(c, debug=(), skip=()):
    nc = bass.Bass("TRN2", target_bir_lowering=False)
    g = Builder(nc)
    D, NT, R, CW, E, FF, NL, CTX, L = c.D, c.NT, c.R, c.CW, c.E, c.FF, c.NL, c.CTX, c.L
    KD, KR, KC, NH = D // 128, R // 128, CW // 128, c.NH
    NCH = NT // 64
    off, NV = vec_layout(c)

    def din(name, shape, dt=F32):
        return nc.dram_tensor(name, list(shape), dt, kind="ExternalInput").ap()

    def dscr(name, shape, dt):
        return nc.dram_tensor(name, list(shape), dt, kind=("ExternalOutput" if name in debug else "Internal")).ap()

    xT0 = din("xT0", [D, NT])
    cs_d = din("cs", [128, KD, 2])
    consts_d = din("consts", [128, 6, 128])
    vecs_d = din("vecs", [NL, 128, NV])
    Wd = dict(w_mod=din("w_mod", [NL, D, 6 * D]), w_in=din("w_in", [NL, D, c.INC]), w2a2=din("w2a2", [NL, 2, 128, R]),
              g2=din("g2", [NL, 128, R]), w_rwkv_out=din("w_rwkv_out", [NL, R, D]), w_conv_out=din("w_conv_out", [NL, CW, D]),
              w_o=din("w_o", [NL, D, D]), w_router=din("w_router", [NL, D, E]), w_exp_gate=din("w_exp_gate", [NL, E, D, FF]),
              w_exp_up=din("w_exp_up", [NL, E, D, FF]), w_exp_down=din("w_exp_down", [NL, E, FF, D]))
    outT = nc.dram_tensor("outT", [D, L], F32, kind="ExternalOutput").ap()

    xres = dscr("xres", [D, NT], F32)
    hT = dscr("hT", [D, NT], BF16)
    PT = dscr("PT", [c.INC, NT], BF16)
    QR = [dscr(f"QR{d}", [NCH, 128, KR, 2, 64], BF16) for d in range(2)]
    BK = [dscr(f"BK{d}", [NCH, 128, KR, 2, 64], BF16) for d in range(2)]
    VT = [dscr(f"VT{d}", [3, R, NT], BF16) for d in range(2)]
    VM = [dscr(f"VM{d}", [3, NT, R], BF16) for d in range(2)]
    BON = [dscr(f"BON{d}", [R, NT], F32) for d in range(2)]
    Yd = [dscr(f"Y{d}", [NT, R], F32) for d in range(2)]
    YT = [dscr(f"YT{d}", [R, NT], F32) for d in range(2)]
    GT = dscr("GT", [R, NT], BF16)
    YG = dscr("YG", [R, NT], BF16)
    BR = dscr("BR", [D, NT], BF16)
    SC = dscr("SC", [CW, NT], BF16)
    BC = dscr("BC", [D, NT], BF16)
    ZT = dscr("ZT", [D, NT], BF16)
    MT = dscr("MT", [D, NT], F32)
    h2T = dscr("h2T", [D, NT], BF16)
    H2 = dscr("H2", [NT, D], BF16)
    AFF = dscr("AFF", [E, NT], F32)
    SL = c.CAP_C + c.CAP_L
    IDX = dscr("IDX", [E, SL], U32)
    VAL = dscr("VAL", [E, SL], F32)
    XST = dscr("XST", [E, D, SL], BF16)
    GH = dscr("GH", [E, FF, SL], BF16)
    UH = dscr("UH", [E, FF, SL], BF16)
    YET = dscr("YET", [E, D, SL], F32)
    MO = dscr("MO", [NT, D], F32)
    MOT = dscr("MOT", [D, NT], F32)

    cst = g.sb("cst", [128, 6, 128], F32, stage=False)
    cstb = g.sb("cstb", [128, 6, 128], BF16, stage=False)
    vecs = g.sb("vecs", [128, NV], F32, stage=False)
    csil = g.sb("csil", [128, KD, 2], F32, stage=False)
    modT = g.sb("modT", [128, 6 * KD, 2], F32, stage=False)
    sc1 = g.sb("sc1", [128, KD, 2], F32, stage=False)
    sc2 = g.sb("sc2", [128, KD, 2], F32, stage=False)
    omka = g.sb("omka", [128, 2, KR], F32, stage=False)
    GC = g.sb("GC", [128, 2, KR, NCH], F32, stage=False)
    ld(g, "sp", cst[:, :, :], consts_d)
    cp(g, "dve", cstb[:, :, :], cst[:, :, :])
    ld(g, "sp", csil[:, :, :], cs_d)
    act(g, csil[:, :, :], csil[:, :, :], AF.Silu)
    ident, ones, bo = cst[:, 0, :], cst[:, 1, :], cst[:, 2, :]
    identb, bob = cstb[:, 0, :], cstb[:, 2, :]

    def V_(name, i=0, n=1):
        return vecs[:, off[name] + i:off[name] + i + n]

    ctx_tiles = [(t0, min(512, CTX - t0), 1) for t0 in range(0, CTX, 512)]
    lat_tiles = [(CTX + t0, 512, 0) for t0 in range(0, L, 512)]
    all_tiles = ctx_tiles + lat_tiles

    def stage_mods(l):
        with g.stage():
            ld(g, "sp", vecs[:, :], vecs_d[l])
            wr = g.pool_of(2, "mw", [128, KD, 512], F32)
            psr = g.pool_of(2, "mp", [128, 4, 2], F32, kind="ps")
            for nb in range(6 * D // 512):
                w = wr.next()
                ld(g, "sp", w[:, :, :], Wd["w_mod"][l][:, nb * 512:(nb + 1) * 512].rearrange("(kc p) n -> p kc n", p=128))
                ps = psr.next()
                for j in range(4):
                    for kc in range(KD):
                        mm(g, ps[:, j, :], w[:, kc, j * 128:(j + 1) * 128], csil[:, kc, :], kc == 0, kc == KD - 1)
                for j2 in range(2):
                    tt(g, "dve", modT[:, nb * 4:nb * 4 + 4, j2], ps[:, :, j2], V_("bmod", nb * 4, 4), ALU.add)
            for j2 in range(2):
                stt(g, "dve", sc1[:, :, j2], modT[:, KD:2 * KD, j2], 1.0, V_("n1g", 0, KD), ALU.add, ALU.mult)
                stt(g, "dve", sc2[:, :, j2], modT[:, 4 * KD:5 * KD, j2], 1.0, V_("n2g", 0, KD), ALU.add, ALU.mult)
            for d in range(2):
                ts(g, "dve", omka[:, d, :], V_(f"ka{d}", 0, KR), -1.0, ALU.mult, 1.0, ALU.add)

    def stage_norm(src, dst, scv, sh_off, tiles, router_l=None, final=False):
        with g.stage():
            xr = g.pool_of(2, "nx", [128, KD, 512], F32)
            sq = g.sb("nsq", [128, KD, 512], F32)
            rs = g.sb("nrs", [128, 512], F32)
            hr = g.pool_of(2, "nh", [128, KD, 512], F32 if final else BF16)
            ps = g.ps("nps", [128, 512])
            if router_l is not None:
                wrt = g.sb("nwr", [128, KD, E], F32)
                ld(g, "sp", wrt[:, :, :], Wd["w_router"][router_l].rearrange("(kc p) e -> p kc e", p=128))
                hf = g.sb("nhf", [128, KD, 512], F32)
                ps2 = g.ps("nps2", [E, 512])
                ps3 = g.ps("nps3", [E, 512])
                ex = g.sb("nex", [E, 512], F32)
                af = g.sb("naf", [E, 512], F32)
            for (t0, tsz, j) in tiles:
                x = xr.next()
                ld(g, "sp", x[:, :, :tsz], src[:, t0:t0 + tsz].rearrange("(kc p) t -> p kc t", p=128))
                act(g, sq[:, :, :tsz], x[:, :, :tsz], AF.Square)
                for kc in range(KD):
                    mm(g, ps[:, :tsz], ones, sq[:, kc, :tsz], kc == 0, kc == KD - 1)
                act(g, rs[:, :tsz], ps[:, :tsz], AF.Sqrt, bias=1e-6, scale=1.0 / D)
                recip(g, rs[:, :tsz], rs[:, :tsz])
                h = hr.next()
                for kc in range(KD):
                    tt(g, "dve", x[:, kc, :tsz], x[:, kc, :tsz], rs[:, :tsz], ALU.mult)
                    if final:
                        act(g, h[:, kc, :tsz], x[:, kc, :tsz], AF.Identity, scale=V_("fin", kc))
                    elif router_l is not None:
                        act(g, hf[:, kc, :tsz], x[:, kc, :tsz], AF.Identity, bias=modT[:, sh_off + kc, j:j + 1], scale=scv[:, kc, j:j + 1])
                        cp(g, "pool", h[:, kc, :tsz], hf[:, kc, :tsz])
                    else:
                        act(g, h[:, kc, :tsz], x[:, kc, :tsz], AF.Identity, bias=modT[:, sh_off + kc, j:j + 1], scale=scv[:, kc, j:j + 1])
                if final:
                    st(g, "pool", dst[:, t0 - CTX:t0 - CTX + tsz].rearrange("(kc p) t -> p kc t", p=128), h[:, :, :tsz])
                else:
                    st(g, "pool", dst[:, t0:t0 + tsz].rearrange("(kc p) t -> p kc t", p=128), h[:, :, :tsz])
                if router_l is not None:
                    for kc in range(KD):
                        mm(g, ps2[:, :tsz], wrt[:, kc, :], hf[:, kc, :tsz], kc == 0, kc == KD - 1)
                    act(g, ex[:, :tsz], ps2[:, :tsz], AF.Exp)
                    mm(g, ps3[:, :tsz], cst[0:E, 1, 0:E], ex[:, :tsz])
                    recip(g, af[:, :tsz], ps3[:, :tsz])
                    tt(g, "dve", af[:, :tsz], af[:, :tsz], ex[:, :tsz], ALU.mult)
                    st(g, "pool", AFF[:, t0:t0 + tsz], af[:, :tsz])

    def stage_prep(l, d):
        offw = c.OFF_WA_F if d == 0 else c.OFF_WA_B
        mask_unused = None
        with g.stage():
            w2a2 = g.sb("pw", [128, R], BF16)
            w2f = g.sb("pwf", [128, R], F32)
            ld(g, "sp", w2f[:, :], Wd["w2a2"][l, d])
            cp(g, "dve", w2a2[:, :], w2f[:, :])
            zb = g.pool_of(2, "pz", [128, 4, 513], BF16)
            f = {n: g.sb("p" + n, [128, 512], F32) for n in
                 ("r", "k", "v", "wa", "d", "sg", "a", "kk", "ss", "km", "bv", "Lc", "Le", "TB", "X3", "X4", "g1", "g2", "g3", "g4", "tmp")}
            wab = g.sb("pwab", [128, 512], BF16)
            sqb = g.sb("psqb", [128, 512], BF16)
            tot = g.sb("ptot", [128, 8], F32)
            psA = g.ps("ppa", [128, 512]); psB = g.ps("ppb", [128, 512]); psC = g.ps("ppc", [128, 512]); psD = g.ps("ppd", [128, 512])
            qro = g.pool_of(2, "pqr", [128, 8, KR, 2, 64], BF16)
            bko = g.pool_of(2, "pbk", [128, 8, KR, 2, 64], BF16)
            vo = g.pool_of(2, "pvo", [128, 3, 512], BF16)
            bono = g.pool_of(2, "pbo", [128, 512], F32)
            for (t0, tsz, j) in all_tiles:
                nch = tsz // 64
                ch0 = t0 // 64
                seq0, seq1 = (0, CTX) if j == 1 else (CTX, NT)
                def load_shift(zt, slot, row0):
                    if d == 0:
                        if t0 == seq0:
                            mset(g, "pool", zt[:, slot, 0:1], 0.0)
                            ld(g, "sp", zt[:, slot, 1:tsz + 1], PT[row0:row0 + 128, t0:t0 + tsz])
                        else:
                            ld(g, "sp", zt[:, slot, 0:tsz + 1], PT[row0:row0 + 128, t0 - 1:t0 + tsz])
                        return zt[:, slot, 1:tsz + 1], zt[:, slot, 0:tsz]
                    else:
                        if t0 + tsz == seq1:
                            mset(g, "pool", zt[:, slot, tsz:tsz + 1], 0.0)
                            ld(g, "sp", zt[:, slot, 0:tsz], PT[row0:row0 + 128, t0:t0 + tsz])
                        else:
                            ld(g, "sp", zt[:, slot, 0:tsz + 1], PT[row0:row0 + 128, t0:t0 + tsz + 1])
                        return zt[:, slot, 0:tsz], zt[:, slot, 1:tsz + 1]

                def mix(out, z, zs, mu):
                    tt(g, "dve", f["d"][:, :tsz], zs, z, ALU.subtract)
                    stt(g, "dve", out, f["d"][:, :tsz], mu, z, ALU.mult, ALU.add)
                zt = zb.next()
                z, zs = load_shift(zt, 3, offw)
                mix(f["wa"][:, :tsz], z, zs, V_(f"mu_wa{d}"))
                act(g, wab[0:64, :tsz], f["wa"][0:64, :tsz], AF.Tanh)
                cp(g, "pool", wab[64:128, :tsz], f["wa"][64:128, :tsz])
                qo, bo_ = qro.next(), bko.next()
                for cc in range(KR):
                    zt = zb.next()
                    z, zs = load_shift(zt, 0, 0 * R + cc * 128); mix(f["r"][:, :tsz], z, zs, V_(f"mu_r{d}", cc))
                    z, zs = load_shift(zt, 1, 1 * R + cc * 128); mix(f["k"][:, :tsz], z, zs, V_(f"mu_k{d}", cc))
                    z, zs = load_shift(zt, 2, 2 * R + cc * 128); mix(f["v"][:, :tsz], z, zs, V_(f"mu_v{d}", cc))
                    mm(g, psA[:, :tsz], w2a2[0:64, cc * 128:(cc + 1) * 128], wab[0:64, :tsz])
                    mm(g, psB[:, :tsz], w2a2[64:128, cc * 128:(cc + 1) * 128], wab[64:128, :tsz])
                    act(g, f["sg"][:, :tsz], psA[:, :tsz], AF.Sigmoid, bias=V_(f"w0{d}", cc))
                    act(g, f["a"][:, :tsz], psB[:, :tsz], AF.Sigmoid, bias=V_(f"a0{d}", cc))
                    ts(g, "dve", f["kk"][:, :tsz], f["k"][:, :tsz], V_(f"kk{d}", cc), ALU.mult)
                    tt(g, "pool", sqb[:, :tsz], f["kk"][:, :tsz], f["kk"][:, :tsz], ALU.mult)
                    mm(g, psC[:, :tsz], bob, sqb[:, :tsz])
                    act(g, f["ss"][:, :tsz], psC[:, :tsz], AF.Sqrt)
                    ts(g, "dve", f["ss"][:, :tsz], f["ss"][:, :tsz], 1e-12, ALU.max)
                    recip(g, f["ss"][:, :tsz], f["ss"][:, :tsz])
                    tt(g, "dve", f["kk"][:, :tsz], f["kk"][:, :tsz], f["ss"][:, :tsz], ALU.mult)
                    ts(g, "dve", f["tmp"][:, :tsz], f["a"][:, :tsz], V_(f"ka{d}", cc), ALU.mult, omka[:, d, cc:cc + 1], ALU.add)
                    tt(g, "dve", f["km"][:, :tsz], f["k"][:, :tsz], f["tmp"][:, :tsz], ALU.mult)
                    tt(g, "pool", f["bv"][:, :tsz], f["kk"][:, :tsz], f["a"][:, :tsz], ALU.mult)
                    stt(g, "dve", sqb[:, :tsz], f["r"][:, :tsz], V_("rk", cc), f["km"][:, :tsz], ALU.mult, ALU.mult)
                    mm(g, psD[:, :tsz], bob, sqb[:, :tsz])
                    bn = bono.next()
                    tt(g, "dve", bn[:, :tsz], psD[:, :tsz], f["v"][:, :tsz], ALU.mult)
                    st(g, "pool", BON[d][cc * 128:(cc + 1) * 128, t0:t0 + tsz], bn[:, :tsz])
                    for ci in range(nch):
                        sl = slice(ci * 64, (ci + 1) * 64)
                        g.op("dve", lambda h, sl=sl: h.tensor_tensor_scan(out=f["Lc"].ap[:, sl], data0=cst.ap[:, 1, 0:64], data1=f["sg"].ap[:, sl],
                                                                          initial=0.0, op0=ALU.mult, op1=ALU.add),
                             [f["sg"], cst], [f["Lc"]])
                    tt(g, "pool", f["Le"][:, :tsz], f["Lc"][:, :tsz], f["sg"][:, :tsz], ALU.subtract)
                    lc3 = f["Lc"][:, :tsz].re("p (c t) -> p c t", t=64)
                    cp(g, "dve", tot[:, :nch], lc3[:, :, 63])
                    act(g, GC[:, d, cc, ch0:ch0 + nch], tot[:, :nch], AF.Exp, scale=-C0)
                    for ci in range(nch):
                        ts(g, "pool", f["TB"][:, ci * 64:(ci + 1) * 64], f["Lc"][:, ci * 64:(ci + 1) * 64], 0.0, ALU.mult, tot[:, ci:ci + 1], ALU.add)
                    tt(g, "dve", f["X3"][:, :tsz], f["TB"][:, :tsz], f["Lc"][:, :tsz], ALU.subtract)
                    tt(g, "pool", f["X4"][:, :tsz], f["TB"][:, :tsz], f["Le"][:, :tsz], ALU.subtract)
                    if d == 0:
                        spec = (("Lc", -C0), ("Le", -C0), ("Lc", C0), ("X3", -C0))
                    else:
                        spec = (("X4", -C0), ("X3", -C0), ("X4", C0), ("Le", -C0))
                    for gi, (srcn, scl) in enumerate(spec):
                        act(g, f[f"g{gi + 1}"][:, :tsz], f[srcn][:, :tsz], AF.Exp, scale=scl)

                    def c3(v):
                        return v.re("p (c t) -> p c t", t=64)
                    tt(g, "dve", qo[:, :nch, cc, 0, :], c3(f["kk"][:, :tsz]), c3(f["g2"][:, :tsz]), ALU.mult)
                    tt(g, "pool", qo[:, :nch, cc, 1, :], c3(f["r"][:, :tsz]), c3(f["g1"][:, :tsz]), ALU.mult)
                    tt(g, "dve", bo_[:, :nch, cc, 0, :], c3(f["bv"][:, :tsz]), c3(f["g3"][:, :tsz]), ALU.mult)
                    tt(g, "pool", bo_[:, :nch, cc, 1, :], c3(f["km"][:, :tsz]), c3(f["g3"][:, :tsz]), ALU.mult)
                    vv = vo.next()
                    cp(g, "act", vv[:, 0, :tsz], f["v"][:, :tsz])
                    tt(g, "dve", vv[:, 1, :tsz], f["bv"][:, :tsz], f["g4"][:, :tsz], ALU.mult)
                    tt(g, "pool", vv[:, 2, :tsz], f["km"][:, :tsz], f["g4"][:, :tsz], ALU.mult)
                    st(g, "pool", VT[d][:, cc * 128:(cc + 1) * 128, t0:t0 + tsz].rearrange("q p t -> p q t"), vv[:, :, :tsz])
                st(g, "act", QR[d][ch0:ch0 + nch].rearrange("c p k w t -> p c (k w t)"), qo[:, :nch].re("p c k w t -> p c (k w t)"))
                st(g, "act", BK[d][ch0:ch0 + nch].rearrange("c p k w t -> p c (k w t)"), bo_[:, :nch].re("p c k w t -> p c (k w t)"))
        for q in range(3):
            transpose_dram(g, VT[d][q], R, NT, VM[d][q], BF16, identb)

    def stage_scan2():
        IDT = BF16
        HH = max(1, NH // 2)
        halves = [list(range(h0, min(NH, h0 + HH))) for h0 in range(0, NH, HH)]
        with g.stage():
            S = []
            for d in range(2):
                r = {}
                r["Hs"] = g.sb(f"sH{d}", [128, KR, 64], F32)
                r["Hb2"] = g.sb(f"sHb2{d}", [128, NH, 64], BF16)
                mset(g, "dve", r["Hs"][:, :, :], 0.0)
                mset(g, "pool", r["Hb2"][:, :, :], 0.0)
                r["qrr"] = g.pool_of(2, f"sqr{d}", [128, KR, 2, 64], BF16)
                r["bkr"] = g.pool_of(2, f"sbk{d}", [128, KR, 2, 64], BF16)
                r["uvr"] = g.pool_of(2, f"suv{d}", [128, R], BF16)
                r["bpr"] = g.pool_of(2, f"sbp{d}", [128, R], BF16)
                zvs = [g.sb(f"szv{d}{i}", [128, R], BF16) for i in range(2)]
                for zv_ in zvs:
                    mset(g, "pool", zv_[:, :], 0.0)
                r["zvr"] = Ring(zvs)
                r["Gs"] = g.sb(f"sG{d}", [128, NH, 128], BF16)
                r["P"] = [g.sb(f"sP{d}{i}", [64, NH, 2, 64], IDT) for i in range(2)]
                r["Pt"] = [g.sb(f"sPt{d}{i}", [64, NH, 64], IDT) for i in range(2)]
                r["X"] = [g.sb(f"sX{d}{i}", [64, NH, 64], F32) for i in range(2)]
                r["Xb"] = g.sb(f"sXb{d}", [64, NH, 64], IDT)
                r["Wsb"] = g.sb(f"sW{d}", [64, NH, 64], BF16)
                r["ysr"] = g.pool_of(2, f"sy{d}", [64, R], F32)
                r["psG"] = g.ps(f"spG{d}", [128, HH, 128])
                r["psB"] = g.ps(f"spB{d}", [64, HH, 64])
                r["psC"] = g.ps(f"spC{d}", [64, HH, 64])
                S.append(r)
            eyeI = g.sb("seye", [64, NH, 64], F32)
            for h_ in range(NH):
                cp(g, "pool", eyeI[:, h_, :], cst[0:64, 0, 0:64])

            def chunk_gen(d, ch):
                r = S[d]
                maskG = cst[:, 3 + d, :]
                maskA = cst[0:64, 4 - d, 0:64]
                Hs, Hb2, Gs, P_, Pt, X_, Xb, Wsb = r["Hs"], r["Hb2"], r["Gs"], r["P"], r["Pt"], r["X"], r["Xb"], r["Wsb"]
                psG, psB, psC = r["psG"], r["psB"], r["psC"]
                t0 = ch * 64
                qr, bk, uv, bp, zv = r["qrr"].next(), r["bkr"].next(), r["uvr"].next(), r["bpr"].next(), r["zvr"].next()
                ld(g, "sp", qr[:, :, :, :], QR[d][ch])
                ld(g, "sp", bk[:, :, :, :], BK[d][ch])
                ld(g, "sp", uv[64:128, :], VM[d][0][t0:t0 + 64, :])
                ld(g, "sp", zv[64:128, :], VM[d][0][t0:t0 + 64, :])
                ld(g, "sp", bp[0:64, :], VM[d][1][t0:t0 + 64, :])
                ld(g, "sp", bp[64:128, :], VM[d][2][t0:t0 + 64, :])
                yield
                for hs in halves:
                    h0, n = hs[0], len(hs)
                    hsl = slice(h0, h0 + n)
                    for i, h_ in enumerate(hs):
                        p0, cc = (h_ % 2) * 64, h_ // 2
                        mm(g, psG[:, i, :], bk[p0:p0 + 64, cc, :, :].re("p w t -> p (w t)"), qr[p0:p0 + 64, cc, :, :].re("p w t -> p (w t)"))
                        mm(g, psB[:, i, :], qr[p0:p0 + 64, cc, 0, :], bk[p0:p0 + 64, cc, 0, :])
                    tt(g, "dve", Gs[:, hsl, :], psG[:, :n, :], maskG.bc3(n), ALU.mult)
                    stt(g, "dve", Pt[0][:, hsl, :], psB[:, :n, :], -1.0, maskA.bc3(n), ALU.mult, ALU.mult)
                    ts(g, "pool", P_[0][:, hsl, 0, :], Gs[0:64, hsl, 0:64], -1.0, ALU.mult)
                    tt(g, "dve", X_[0][:, hsl, :], eyeI[:, hsl, :], P_[0][:, hsl, 0, :], ALU.add)
                    yield
                for hs in halves:
                    h0, n = hs[0], len(hs)
                    hsl = slice(h0, h0 + n)
                    for i, h_ in enumerate(hs):
                        mm(g, psC[:, i, :], P_[0][:, h_, 0, :], Pt[0][:, h_, :])
                        mm(g, psB[:, i, :], Pt[0][:, h_, :], P_[0][:, h_, 0, :])
                    cp(g, "act", Pt[1][:, hsl, :], psC[:, :n, :])
                    cp(g, "dve", P_[1][:, hsl, 0, :], psB[:, :n, :])
                    cp(g, "pool", P_[1][:, hsl, 1, :], X_[0][:, hsl, :])
                    yield
                cur, xc = 1, 0
                for jj in range(1, 6):
                    nxt, xn = 1 - cur, 1 - xc
                    for hs in halves:
                        h0, n = hs[0], len(hs)
                        hsl = slice(h0, h0 + n)
                        for i, h_ in enumerate(hs):
                            if jj < 5:
                                mm(g, psG[0:64, i, :], Pt[cur][:, h_, :], P_[cur][:, h_, :, :].re("p w t -> p (w t)"))
                                mm(g, psC[:, i, :], P_[cur][:, h_, 0, :], Pt[cur][:, h_, :])
                            else:
                                mm(g, psG[0:64, i, 64:128], Pt[cur][:, h_, :], P_[cur][:, h_, 1, :])
                        if jj < 5:
                            cp(g, "act", Pt[nxt][:, hsl, :], psC[:, :n, :])
                            cp(g, "dve", P_[nxt][:, hsl, 0, :], psG[0:64, :n, 0:64])
                        tt(g, "dve", X_[xn][:, hsl, :], X_[xc][:, hsl, :], psG[0:64, :n, 64:128], ALU.add)
                        if jj < 5:
                            cp(g, "pool", P_[nxt][:, hsl, 1, :], X_[xn][:, hsl, :])
                        else:
                            cp(g, "pool", Xb[:, hsl, :], X_[xn][:, hsl, :])
                        yield
                    cur, xc = nxt, xn
                ys = r["ysr"].next()
                for hs in halves:
                    h0, n = hs[0], len(hs)
                    hsl = slice(h0, h0 + n)
                    csl = slice(h0 * 64, (h0 + n) * 64)
                    for i, h_ in enumerate(hs):
                        cc = h_ // 2
                        mm(g, psB[:, i, :], qr[:, cc, 0, :], Hb2[:, h_, :], True, False)
                        mm(g, psB[:, i, :], Gs[:, h_, 0:64], zv[:, h_ * 64:(h_ + 1) * 64], False, True)
                    cp(g, "act", Wsb[:, hsl, :], psB[:, :n, :])
                    yield
                    for i, h_ in enumerate(hs):
                        mm(g, psC[:, i, :], Xb[:, h_, :], Wsb[:, h_, :])
                    act(g, uv[0:64, csl].re("p (h v) -> p h v", v=64), psC[:, :n, :], AF.Identity, scale=-1.0)
                    yield
                    for i, h_ in enumerate(hs):
                        cc = h_ // 2
                        mm(g, psB[:, i, :], qr[:, cc, 1, :], Hb2[:, h_, :], True, False)
                        mm(g, psB[:, i, :], Gs[:, h_, 64:128], uv[:, h_ * 64:(h_ + 1) * 64], False, True)
                    cp(g, "dve", ys[:, csl].re("p (h v) -> p h v", v=64), psB[:, :n, :])
                    for i, h_ in enumerate(hs):
                        cc = h_ // 2
                        mm(g, psG[:, i, 0:64], bp[:, cc * 128:(cc + 1) * 128], uv[:, h_ * 64:(h_ + 1) * 64])
                    yield
                    for i, h_ in enumerate(hs):
                        par, cc = h_ % 2, h_ // 2
                        pr = slice(par * 64, par * 64 + 64)
                        stt(g, "dve", Hs[pr, cc, :], Hs[pr, cc, :], GC[pr, d, cc, ch:ch + 1], psG[pr, i, 0:64], ALU.mult, ALU.add)
                    yield
                st(g, "pool", Yd[d][t0:t0 + 64, :], ys[:, :])
                cp(g, "pool", Hb2[0:64, 0::2, :], Hs[0:64, :, :])
                if NH > 1:
                    cp(g, "pool", Hb2[64:128, 1::2, :], Hs[64:128, :, :])

            ctx_ch = list(range(CTX // 64))
            lat_ch = list(range(CTX // 64, NCH))
            orders = [ctx_ch + lat_ch, ctx_ch[::-1] + lat_ch[::-1]]
            for i in range(NCH):
                gens = [chunk_gen(0, orders[0][i]), chunk_gen(1, orders[1][i])]
                while gens:
                    for gn in list(gens):
                        try:
                            next(gn)
                        except StopIteration:
                            gens.remove(gn)
        for d in range(2):
            transpose_dram(g, Yd[d], NT, R, YT[d], F32, ident)

    def stage_post(tiles):
        with g.stage():
            ya = g.pool_of(2, "oa", [128, 512], F32); yb = g.pool_of(2, "ob", [128, 512], F32)
            b0 = g.pool_of(2, "o0", [128, 512], F32); b1 = g.pool_of(2, "o1", [128, 512], F32)
            gt = g.pool_of(2, "og", [128, 512], BF16)
            dd = g.sb("od", [128, 512], F32); sq = g.sb("osq", [128, 512], F32); rs = g.sb("ors", [128, 512], F32)
            out = g.pool_of(2, "oo", [128, 512], BF16)
            ps1 = g.ps("op1", [128, 512]); ps2 = g.ps("op2", [128, 512])
            for (t0, tsz, j) in tiles:
                for cc in range(KR):
                    rows = slice(cc * 128, (cc + 1) * 128)
                    a, b, c0_, c1_, gg = ya.next(), yb.next(), b0.next(), b1.next(), gt.next()
                    ld(g, "sp", a[:, :tsz], YT[0][rows, t0:t0 + tsz]); ld(g, "sp", b[:, :tsz], YT[1][rows, t0:t0 + tsz])
                    ld(g, "sp", c0_[:, :tsz], BON[0][rows, t0:t0 + tsz]); ld(g, "sp", c1_[:, :tsz], BON[1][rows, t0:t0 + tsz])
                    ld(g, "sp", gg[:, :tsz], GT[rows, t0:t0 + tsz])
                    tt(g, "dve", a[:, :tsz], a[:, :tsz], b[:, :tsz], ALU.add)
                    mm(g, ps1[:, :tsz], bo, a[:, :tsz])
                    stt(g, "dve", dd[:, :tsz], ps1[:, :tsz], -1.0 / 64, a[:, :tsz], ALU.mult, ALU.add)
                    act(g, sq[:, :tsz], dd[:, :tsz], AF.Square)
                    mm(g, ps2[:, :tsz], bo, sq[:, :tsz])
                    act(g, rs[:, :tsz], ps2[:, :tsz], AF.Sqrt, bias=64e-5, scale=1.0 / 64)
                    recip(g, rs[:, :tsz], rs[:, :tsz])
                    tt(g, "dve", dd[:, :tsz], dd[:, :tsz], rs[:, :tsz], ALU.mult)
                    ts(g, "dve", dd[:, :tsz], dd[:, :tsz], V_("lng", cc), ALU.mult, V_("lnb", cc), ALU.add)
                    tt(g, "pool", c0_[:, :tsz], c0_[:, :tsz], c1_[:, :tsz], ALU.add)
                    tt(g, "dve", dd[:, :tsz], dd[:, :tsz], c0_[:, :tsz], ALU.add)
                    o = out.next()
                    tt(g, "dve", o[:, :tsz], dd[:, :tsz], gg[:, :tsz], ALU.mult)
                    st(g, "pool", YG[rows, t0:t0 + tsz], o[:, :tsz])

    def stage_conf(tiles):
        with g.stage():
            ua = g.pool_of(2, "ca", [128, 512], BF16); ub = g.pool_of(2, "cb", [128, 512], BF16)
            u = g.sb("cu", [128, 512], F32)
            acc = g.sb("cacc", [128, KC, 512], F32)
            accbr = g.pool_of(2, "caccb", [128, 512], F32)
            tmr = g.pool_of(3, "ctm", [128, 512], F32)
            sq = g.sb("csq", [128, 512], F32); mean = g.sb("cmean", [128, 512], F32); rs = g.sb("crs", [128, 512], F32)
            out = g.pool_of(2, "co", [128, KC, 512], BF16)
            ps1 = g.ps("cp1", [128, 512]); ps2 = g.ps("cp2", [128, 512])
            for (t0, tsz, j) in tiles:
                rl = CTX if j == 1 else c.GW
                nr = tsz // rl
                for cc in range(KC):
                    a, b = ua.next(), ub.next()
                    ld(g, "sp", a[:, :tsz], PT[c.OFF_GLU + cc * 128:c.OFF_GLU + (cc + 1) * 128, t0:t0 + tsz])
                    ld(g, "sp", b[:, :tsz], PT[c.OFF_GLU + CW + cc * 128:c.OFF_GLU + CW + (cc + 1) * 128, t0:t0 + tsz])
                    tt(g, "pool", u[:, :tsz], a[:, :tsz], b[:, :tsz], ALU.mult)
                    ts(g, "dve", acc[:, cc, :tsz], u[:, :tsz], V_("convw", cc * 31 + 15), ALU.mult, V_("convb", cc), ALU.add)
                    u3 = u[:, :tsz].re("p (r t) -> p r t", t=rl)
                    a3 = acc[:, cc, :tsz].re("p (r t) -> p r t", t=rl)
                    accb = accbr.next()
                    mset(g, "pool", accb[:, :tsz], 0.0)
                    b3 = accb[:, :tsz].re("p (r t) -> p r t", t=rl)
                    for k in range(31):
                        s_ = k - 15
                        if s_ == 0 or abs(s_) >= rl:
                            continue
                        lo, hi = max(0, -s_), rl - max(0, s_)
                        if k < 15:
                            stt(g, "dve", a3[:, :, lo:hi], u3[:, :, lo + s_:hi + s_], V_("convw", cc * 31 + k), a3[:, :, lo:hi], ALU.mult, ALU.add)
                        else:
                            tm = tmr.next()
                            t3 = tm[:, :tsz].re("p (r t) -> p r t", t=rl)
                            act(g, t3[:, :, lo:hi], u3[:, :, lo + s_:hi + s_], AF.Identity, scale=V_("convw", cc * 31 + k))
                            tt(g, "pool", b3[:, :, lo:hi], b3[:, :, lo:hi], t3[:, :, lo:hi], ALU.add)
                    tt(g, "dve", acc[:, cc, :tsz], acc[:, cc, :tsz], accb[:, :tsz], ALU.add)
                for cc in range(KC):
                    mm(g, ps1[:, :tsz], ones, acc[:, cc, :tsz], cc == 0, cc == KC - 1)
                ts(g, "dve", mean[:, :tsz], ps1[:, :tsz], 1.0 / CW, ALU.mult)
                for cc in range(KC):
                    tt(g, "dve", acc[:, cc, :tsz], acc[:, cc, :tsz], mean[:, :tsz], ALU.subtract)
                    act(g, sq[:, :tsz], acc[:, cc, :tsz], AF.Square)
                    mm(g, ps2[:, :tsz], ones, sq[:, :tsz], cc == 0, cc == KC - 1)
                act(g, rs[:, :tsz], ps2[:, :tsz], AF.Sqrt, bias=1e-5, scale=1.0 / CW)
                recip(g, rs[:, :tsz], rs[:, :tsz])
                o = out.next()
                for cc in range(KC):
                    tt(g, "dve", acc[:, cc, :tsz], acc[:, cc, :tsz], rs[:, :tsz], ALU.mult)
                    ts(g, "pool", acc[:, cc, :tsz], acc[:, cc, :tsz], V_("clg", cc), ALU.mult, V_("clb", cc), ALU.add)
                    act(g, o[:, cc, :tsz], acc[:, cc, :tsz], AF.Silu)
                st(g, "pool", SC[:, t0:t0 + tsz].rearrange("(kc p) t -> p kc t", p=128), o[:, :, :tsz])

    def stage_merge(tiles):
        with g.stage():
            r4 = [g.pool_of(2, f"m{i}", [128, 512], BF16) for i in range(4)]
            t1 = g.sb("mt1", [128, 512], F32); t2 = g.sb("mt2", [128, 512], F32)
            out = g.pool_of(2, "mo", [128, 512], BF16)
            for (t0, tsz, j) in tiles:
                for kc in range(KD):
                    rows = slice(kc * 128, (kc + 1) * 128)
                    gr, gc_, br, bc_ = [r.next() for r in r4]
                    ld(g, "sp", gr[:, :tsz], PT[c.OFF_GATE + kc * 128:c.OFF_GATE + (kc + 1) * 128, t0:t0 + tsz])
                    ld(g, "sp", gc_[:, :tsz], PT[c.OFF_GATE + D + kc * 128:c.OFF_GATE + D + (kc + 1) * 128, t0:t0 + tsz])
                    ld(g, "sp", br[:, :tsz], BR[rows, t0:t0 + tsz]); ld(g, "sp", bc_[:, :tsz], BC[rows, t0:t0 + tsz])
                    tt(g, "dve", t1[:, :tsz], gr[:, :tsz], br[:, :tsz], ALU.mult)
                    tt(g, "pool", t2[:, :tsz], gc_[:, :tsz], bc_[:, :tsz], ALU.mult)
                    o = out.next()
                    tt(g, "dve", o[:, :tsz], t1[:, :tsz], t2[:, :tsz], ALU.add)
                    st(g, "pool", ZT[rows, t0:t0 + tsz], o[:, :tsz])

    def stage_resid(src, upd, gate_off, tiles):
        with g.stage():
            xa = g.pool_of(2, "ra", [128, KD, 512], F32); xb = g.pool_of(2, "rb", [128, KD, 512], F32)
            for (t0, tsz, j) in tiles:
                a, b = xa.next(), xb.next()
                ld(g, "sp", a[:, :, :tsz], src[:, t0:t0 + tsz].rearrange("(kc p) t -> p kc t", p=128))
                ld(g, "sp", b[:, :, :tsz], upd[:, t0:t0 + tsz].rearrange("(kc p) t -> p kc t", p=128))
                for kc in range(KD):
                    stt(g, "dve", a[:, kc, :tsz], b[:, kc, :tsz], modT[:, gate_off + kc, j:j + 1], a[:, kc, :tsz], ALU.mult, ALU.add)
                st(g, "pool", xres[:, t0:t0 + tsz].rearrange("(kc p) t -> p kc t", p=128), a[:, :, :tsz])

    def stage_moe(l, with_ctx):
        sets = ([(0, CTX, c.CAP_C, 0)] if with_ctx else []) + [(CTX, L, c.CAP_L, c.CAP_C)]
        transpose_dram(g, h2T, D, NT, H2, BF16, identb)
        with g.stage():
            aff = g.sb("ea", [E, max(L, CTX)], F32)
            vals = g.sb("ev", [E, SL], F32)
            idx = g.sb("ei", [E, SL], U32)
            z = g.sb("ez", [128, D], F32)
            mset(g, "dve", z[:, :], 0.0)
            for r0 in range(0, NT, 128):
                st(g, "sp", MO[r0:r0 + 128, :], z[:, :])
            for (s0, n, cap, so) in sets:
                ld(g, "sp", aff[:, :n], AFF[:, s0:s0 + n])
                for it in range(cap // 8):
                    v8 = vals[:, so + it * 8:so + it * 8 + 8]
                    g.op("dve", lambda h, v8=v8, n=n: h.max(out=v8.ap, in_=aff.ap[:, :n]), [aff], [vals])
                    g.op("dve", lambda h, v8=v8, n=n, it=it, so=so: h.max_index(out=idx.ap[:, so + it * 8:so + it * 8 + 8], in_max=v8.ap, in_values=aff.ap[:, :n]), [aff, vals], [idx])
                    g.op("dve", lambda h, v8=v8, n=n: h.match_replace(out=aff.ap[:, :n], in_to_replace=v8.ap, in_values=aff.ap[:, :n], imm_value=-1.0), [aff, vals], [aff])
            st(g, "sp", IDX[:, :], idx[:, :])
            st(g, "sp", VAL[:, :], vals[:, :])
        with g.stage():
            icr = g.pool_of(3, "gi", [128, 1], U32)
            xsr = g.pool_of(2, "gx", [128, D], BF16)
            xtr = g.pool_of(2, "gt", [128, KD, 128], BF16)
            psr = g.pool_of(2, "gp", [128, 4, 128], BF16, kind="ps")
            for e in range(E):
                for (s0, n, cap, so) in sets:
                    for b0 in range(0, cap, 128):
                        nb = min(128, cap - b0)
                        ic, xs, xt = icr.next(), xsr.next(), xtr.next()
                        ld(g, "sp", ic[:nb, :], IDX[e, so + b0:so + b0 + nb].rearrange("(p o) -> p o", o=1))
                        src_ap = H2; eoff = s0 * D
                        g.dma("pool", None, None, reads=[ic], writes=[xs],
                              indirect=lambda h, xs=xs, ic=ic, nb=nb, src_ap=src_ap, eoff=eoff: h.indirect_dma_start(
                                  out=xs.ap[:nb, :], out_offset=None, in_=src_ap, in_offset=bass.IndirectOffsetOnAxis(ap=ic.ap[:nb, :], axis=0), element_offset=eoff))
                        for k0 in range(0, KD, 4):
                            ps = psr.next()
                            n4 = min(4, KD - k0)
                            for i in range(n4):
                                tr(g, ps[:, i, :nb], xs[:nb, (k0 + i) * 128:(k0 + i + 1) * 128], identb[:nb, :nb])
                            cp(g, "dve" if (k0 // 4) % 2 else "act", xt[:, k0:k0 + n4, :nb], ps[:, :n4, :nb])
                        st(g, "act", XST[e][:, so + b0:so + b0 + nb].rearrange("(kc p) t -> p kc t", p=128), xt[:, :, :nb])
        sl_tiles = [(so_, min(512, cap - t_)) for (s0, n, cap, so) in sets for t_ in range(0, cap, 512) for so_ in [so + t_]]
        lock = Tile(None, "molock")
        jobs = []
        for e in range(E):
            jobs.append((XST[e], D, Wd["w_exp_gate"][l, e], FF, GH[e], sl_tiles, AF.Silu))
            jobs.append((XST[e], D, Wd["w_exp_up"][l, e], FF, UH[e], sl_tiles, AF.Identity))
        linear_fm_multi(g, jobs, BF16, GN=1024)
        with g.stage():
            gar = g.pool_of(2, "fa", [128, FF // 128, SL], BF16); gbr = g.pool_of(2, "fb", [128, FF // 128, SL], BF16)
            for e in range(E):
                ga, gb = gar.next(), gbr.next()
                ld(g, "sp", ga[:, :, :], GH[e].rearrange("(k p) t -> p k t", p=128)); ld(g, "act", gb[:, :, :], UH[e].rearrange("(k p) t -> p k t", p=128))
                tt(g, "dve" if e % 2 else "pool", ga[:, :, :], ga[:, :, :], gb[:, :, :], ALU.mult)
                st(g, "sp", GH[e].rearrange("(k p) t -> p k t", p=128), ga[:, :, :])
        linear_fm_multi(g, [(GH[e], FF, Wd["w_exp_down"][l, e], D, YET[e], sl_tiles, AF.Identity) for e in range(E)], F32, GN=1024)
        with g.stage():
            icr = g.pool_of(3, "si", [128, 1], U32); vcr = g.pool_of(3, "sv", [128, 1], F32)
            ytr = g.pool_of(2, "sy", [128, KD, 128], F32)
            yor = g.pool_of(3, "so", [128, D], F32)
            psr = g.pool_of(2, "sp", [128, 4, 128], F32, kind="ps")
            for e in range(E):
                for (s0, n, cap, so) in sets:
                    for b0 in range(0, cap, 128):
                        nb = min(128, cap - b0)
                        ic, vc, yt, yo = icr.next(), vcr.next(), ytr.next(), yor.next()
                        ld(g, "sp", ic[:nb, :], IDX[e, so + b0:so + b0 + nb].rearrange("(p o) -> p o", o=1))
                        ld(g, "sp", vc[:nb, :], VAL[e, so + b0:so + b0 + nb].rearrange("(p o) -> p o", o=1))
                        ld(g, "sp", yt[:, :, :nb], YET[e][:, so + b0:so + b0 + nb].rearrange("(kc p) t -> p kc t", p=128))
                        for k0 in range(0, KD, 4):
                            ps = psr.next()
                            n4 = min(4, KD - k0)
                            for i in range(n4):
                                tr(g, ps[:nb, i, :], yt[:, k0 + i, :nb], ident)
                            ts(g, "dve", yo[:nb, k0 * 128:(k0 + n4) * 128].re("p (i f) -> p i f", f=128), ps[:nb, :n4, :], vc[:nb, :], ALU.mult)
                        dst_ap = MO; eoff = s0 * D
                        g.dma("pool", None, None, reads=[ic, yo], writes=[lock],
                              indirect=lambda h, yo=yo, ic=ic, nb=nb, dst_ap=dst_ap, eoff=eoff: h.indirect_dma_start(
                                  out=dst_ap, out_offset=bass.IndirectOffsetOnAxis(ap=ic.ap[:nb, :], axis=0), in_=yo.ap[:nb, :], in_offset=None,
                                  compute_op=ALU.add, element_offset=eoff))
        transpose_dram(g, MO, NT, D, MOT, F32, ident)

    tiles2 = [(t0, tsz) for (t0, tsz, j) in all_tiles]
    for l in range(NL):
        last = l == NL - 1
        post_tiles = lat_tiles if last else all_tiles
        post2 = [(t0, tsz) for (t0, tsz, j) in post_tiles]
        stage_mods(l)
        stage_norm(xT0 if l == 0 else xres, hT, sc1, 0, all_tiles)
        W = Wd["w_in"][l]
        linear_fm(g, hT, D, W[:, 0:c.OFF_GLAT], c.OFF_GLAT, PT[0:c.OFF_GLAT], BF16, tiles2, GN=1024)
        linear_fm(g, hT, D, W[:, c.OFF_GLAT:c.OFF_GLU], c.GL, PT[c.OFF_GLAT:c.OFF_GLU], BF16, post2, func=AF.Sigmoid)
        linear_fm(g, hT, D, W[:, c.OFF_GLU:c.OFF_GLU + CW], CW, PT[c.OFF_GLU:c.OFF_GLU + CW], BF16, post2)
        linear_fm(g, hT, D, W[:, c.OFF_GLU + CW:c.INC], CW + 2 * D, PT[c.OFF_GLU + CW:c.INC], BF16, post2, func=AF.Sigmoid, GN=1024)
        for d in range(2):
            if "prep" not in skip:
                stage_prep(l, d)
        if "scan" not in skip:
            stage_scan2()
        linear_fm(g, PT[c.OFF_GLAT:c.OFF_GLU], c.GL, Wd["g2"][l], R, GT, BF16, post2)
        stage_post(post_tiles)
        linear_fm(g, YG, R, Wd["w_rwkv_out"][l], D, BR, BF16, post2)
        stage_conf(post_tiles)
        linear_fm(g, SC, CW, Wd["w_conv_out"][l], D, BC, BF16, post2)
        stage_merge(post_tiles)
        linear_fm(g, ZT, D, Wd["w_o"][l], D, MT, F32, post2)
        stage_resid(xT0 if l == 0 else xres, MT, 2 * KD, post_tiles)
        stage_norm(xres, h2T, sc2, 3 * KD, post_tiles, router_l=l)
        if "moe" not in skip:
            stage_moe(l, not last)
            stage_resid(xres, MOT, 5 * KD, post_tiles)
    stage_norm(xres, outT, None, 0, lat_tiles, final=True)
    g.finish()
    return nc


def prep_inputs(c, I, b):
    xT0 = np.ascontiguousarray(np.concatenate([I["ctx"][b].T, I["x"][b].T], axis=1))
    cs = np.ascontiguousarray(np.stack([fm(I["c"][b]), fm(I["c_ctx"])], axis=-1))
    m = dict(xT0=xT0, cs=cs, consts=make_consts(), vecs=pack_vecs(c, I))
    m["w2a2"] = np.ascontiguousarray(np.concatenate([I["w2"], I["a2"]], axis=2))
    for n in WNAMES:
        if n != "w2a2":
            m[n] = np.asarray(I[n])
    return m


_NC_CACHE = {}


def kernel(**inputs):
    I = {k: np.asarray(v) for k, v in inputs.items()}
    B, L, D = I["x"].shape
    c = Cfg(D=D, L=L, CTX=I["ctx"].shape[1], R=I["w_rwkv_out"].shape[1], CW=I["w_conv_out"].shape[1],
            E=I["w_router"].shape[2], FF=I["w_exp_gate"].shape[3], NL=I["w_in"].shape[0])
    nc = build(c)
    in_maps = [prep_inputs(c, I, b) for b in range(B)]
    res = run_bass_kernel_spmd(nc, in_maps, core_ids=list(range(B)))
    return np.stack([np.ascontiguousarray(res.results[b]["outT"].T) for b in range(B)], axis=0).astype(np.float32)
```

```python
import contextlib
import numpy as np
import concourse.bass as bass
import concourse.mybir as mybir
from concourse.bass_utils import run_bass_kernel_spmd

F32 = mybir.dt.float32
BF16 = mybir.dt.bfloat16
I32 = mybir.dt.int32
U32 = mybir.dt.uint32
AF = mybir.ActivationFunctionType
ALU = mybir.AluOpType
AX = mybir.AxisListType


import os
SAME_ENG_WAIT = os.environ.get('SAME_ENG_WAIT', '1') == '1'


class Cfg:
    def __init__(s, D=2048, L=8192, CTX=256, R=1024, CW=1024, E=16, FF=1024, NL=4, GW=64):
        s.D, s.L, s.CTX, s.R, s.CW, s.E, s.FF, s.NL, s.GW = D, L, CTX, R, CW, E, FF, NL, GW
        s.HD = 64
        s.NH = R // 64
        s.DL, s.AL, s.GL = 64, 64, 128
        s.CK = 31
        s.NT = CTX + L
        s.OFF_WA_F = 3 * R
        s.OFF_WA_B = s.OFF_WA_F + 128
        s.OFF_GLAT = s.OFF_WA_B + 128
        s.OFF_GLU = s.OFF_GLAT + s.GL
        s.OFF_GATE = s.OFF_GLU + 2 * CW
        s.INC = s.OFF_GATE + 2 * D
        s.CAP_L = 2 * L // E
        s.CAP_C = 2 * CTX // E


class View:
    __slots__ = ("t", "ap")

    def __init__(s, t, ap):
        s.t, s.ap = t, ap

    def __getitem__(s, k):
        return View(s.t, s.ap[k])

    def bc(s, shape):
        return View(s.t, s.ap.to_broadcast(list(shape)))

    def re(s, pat, **kw):
        return View(s.t, s.ap.rearrange(pat, **kw))

    def bc3(s, n):
        sh = list(s.ap.shape)
        return View(s.t, s.ap.unsqueeze(1).to_broadcast([sh[0], n, sh[1]]))


class Tile:
    __slots__ = ("ap", "w", "r", "name")

    def __init__(s, ap, name):
        s.ap, s.name = ap, name
        s.w = None
        s.r = {}

    def __getitem__(s, k):
        return View(s, s.ap[k])


class Builder:
    ENG = ("pe", "act", "dve", "pool", "sp")

    def __init__(s, nc, ndma=8):
        s.nc = nc
        s.h = {"pe": nc.tensor, "act": nc.scalar, "dve": nc.vector, "pool": nc.gpsimd, "sp": nc.sync}
        s.prog = {e: [] for e in s.ENG}
        s.sem = {e: nc.alloc_semaphore(name="s_" + e) for e in s.ENG}
        s.cnt = {e: 0 for e in s.ENG}
        s.waited = {e: {} for e in s.ENG}
        s.dq = {}
        for q in ("sp", "pool", "act"):
            s.dq[q] = dict(sems=[nc.alloc_semaphore(name=f"d_{q}{i}") for i in range(ndma)], n=0)
        s.stack = contextlib.ExitStack()
        s.stage_stack = None
        s.uid = 0

    def _alloc(s, kind, name, shape, dt, stage=True):
        s.uid += 1
        nm = f"{name}_{s.uid}"
        st = s.stage_stack if (stage and s.stage_stack is not None) else s.stack
        if kind == "sb":
            t = st.enter_context(s.nc.sbuf_tensor(nm, list(shape), dt))
        else:
            t = st.enter_context(s.nc.psum_tensor(nm, list(shape), dt))
        return Tile(t, nm)

    def sb(s, name, shape, dt, stage=True):
        return s._alloc("sb", name, shape, dt, stage)

    def ps(s, name, shape, dt=F32, stage=True):
        return s._alloc("ps", name, shape, dt, stage)

    def pool_of(s, n, name, shape, dt, kind="sb"):
        return Ring([s._alloc(kind, name, shape, dt) for _ in range(n)])

    @contextlib.contextmanager
    def stage(s):
        s.barrier()
        old = s.stage_stack
        s.stage_stack = contextlib.ExitStack()
        try:
            yield
        finally:
            s.barrier()
            s.stage_stack.close()
            s.stage_stack = old

    def _wait(s, eng, tok):
        if tok is None:
            return
        sem, val = tok
        key = id(sem)
        if (not SAME_ENG_WAIT) and sem is s.sem.get(eng):
            return
        if s.waited[eng].get(key, 0) >= val:
            return
        s.waited[eng][key] = val
        s.h[eng].wait_ge(sem, val)

    def _deps(s, eng, reads, writes, own_tok_is_pe=False):
        for t in reads:
            s._wait(eng, t.w)
        for t in writes:
            if not (own_tok_is_pe and t.w is not None and t.w[0] is s.sem["pe"]):
                s._wait(eng, t.w)
            for tok in t.r.values():
                s._wait(eng, tok)

    def _mark(s, tok, reads, writes):
        for t in reads:
            t.r[id(tok[0])] = tok
        for t in writes:
            t.w = tok
            t.r = {}

    def op(s, eng, fn, reads=(), writes=()):
        s._deps(eng, reads, writes, own_tok_is_pe=(eng == "pe"))
        s.cnt[eng] += 1
        tok = (s.sem[eng], s.cnt[eng])
        sem = s.sem[eng]
        fn(s.h[eng]).then_inc(sem, 1)
        s._mark(tok, reads, writes)
        return tok

    def dma(s, q, out, in_, reads=(), writes=(), indirect=None, **kw):
        dq = s.dq[q]
        i = dq["n"] % len(dq["sems"])
        gen = dq["n"] // len(dq["sems"])
        dq["n"] += 1
        sem = dq["sems"][i]
        s._wait(q, (sem, 16 * gen))
        s._deps(q, reads, writes)
        tok = (sem, 16 * (gen + 1))
        if indirect is None:
            s.h[q].dma_start(out=out, in_=in_, **kw).then_inc(sem, 16)
        else:
            indirect(s.h[q]).then_inc(sem, 16)
        s._mark(tok, reads, writes)
        return tok

    def barrier(s):
        toks = [(s.sem[e], s.cnt[e]) for e in s.ENG if s.cnt[e] > 0]
        for q, dq in s.dq.items():
            ns = len(dq["sems"])
            for i, sem in enumerate(dq["sems"]):
                used = (dq["n"] - i + ns - 1) // ns
                if used > 0:
                    toks.append((sem, 16 * used))
        for e in s.ENG:
            for tok in toks:
                if tok[0] is s.sem[e]:
                    continue
                s._wait(e, tok)

    def finish(s):
        s.barrier()
        s.stack.close()


class Ring:
    def __init__(s, tiles):
        s.t, s.i = tiles, 0

    def next(s):
        t = s.t[s.i % len(s.t)]
        s.i += 1
        return t


def _tiles(*vs):
    return [v.t for v in vs if isinstance(v, View)]


def _ap(v):
    return v.ap if isinstance(v, View) else v


def tt(g, eng, out, a, b, op):
    g.op(eng, lambda h: h.tensor_tensor(out=out.ap, in0=a.ap, in1=b.ap, op=op), _tiles(a, b), _tiles(out))


def ts(g, eng, out, a, s1, op0, s2=None, op1=None):
    kw = dict(out=out.ap, in0=a.ap, scalar1=_ap(s1), scalar2=_ap(s2), op0=op0)
    if op1 is not None:
        kw["op1"] = op1
    g.op(eng, lambda h: h.tensor_scalar(**kw), _tiles(a, s1, s2), _tiles(out))


def stt(g, eng, out, a, sc, b, op0, op1):
    g.op(eng, lambda h: h.scalar_tensor_tensor(out=out.ap, in0=a.ap, scalar=_ap(sc), in1=b.ap, op0=op0, op1=op1),
         _tiles(a, sc, b), _tiles(out))


def act(g, out, a, func, bias=None, scale=None):
    kw = dict(out=out.ap, in_=a.ap, func=func)
    if bias is not None:
        kw["bias"] = _ap(bias)
    if scale is not None:
        kw["scale"] = _ap(scale)
    g.op("act", lambda h: h.activation(**kw), _tiles(a, bias, scale), _tiles(out))


def mm(g, out, lhsT, rhs, start=True, stop=True):
    g.op("pe", lambda h: h.matmul(out.ap, lhsT.ap, rhs.ap, start=start, stop=stop), _tiles(lhsT, rhs), _tiles(out))


def tr(g, out, a, ident):
    g.op("pe", lambda h: h.transpose(out.ap, a.ap, ident.ap), _tiles(a, ident), _tiles(out))


def cp(g, eng, out, a):
    if eng == "act":
        g.op("act", lambda h: h.copy(out=out.ap, in_=a.ap), _tiles(a), _tiles(out))
    else:
        g.op(eng, lambda h: h.tensor_copy(out=out.ap, in_=a.ap), _tiles(a), _tiles(out))


def recip(g, out, a):
    g.op("dve", lambda h: h.reciprocal(out=out.ap, in_=a.ap), _tiles(a), _tiles(out))


def mset(g, eng, out, val):
    g.op(eng, lambda h: h.memset(out.ap, val), (), _tiles(out))


def ld(g, q, out, dram, **kw):
    g.dma(q, out.ap, dram, reads=(), writes=[out.t], **kw)


def st(g, q, dram, a, **kw):
    g.dma(q, dram, a.ap, reads=[a.t], writes=(), **kw)


class Alt:
    def __init__(s, engs):
        s.e, s.i = engs, 0

    def __call__(s):
        s.i += 1
        return s.e[s.i % len(s.e)]


def linear_fm(g, XT, K, W, N, YT, ydt, tiles, func=AF.Identity, GN=512):
    linear_fm_multi(g, [(XT, K, W, N, YT, tiles, func)], ydt, GN=GN)


def linear_fm_multi(g, jobs, ydt, GN=512):
    KCm = max(j[1] for j in jobs) // 128
    GN = min(GN, max(j[3] for j in jobs))
    with g.stage():
        wbr = g.pool_of(2, "lw", [128, KCm, GN], BF16)
        wsr = g.pool_of(3, "lws", [128, GN], F32)
        xr = g.pool_of(2, "lx", [128, KCm, 512], BF16)
        yr = g.pool_of(2, "ly", [128, GN // 128, 512], ydt)
        psr = g.pool_of(4, "lps", [128, 512], F32, kind="ps")
        ce = Alt(["dve", "pool"])
        ee = Alt(["act", "dve"])

        def load_w(grp):
            (XT, K, W, N, YT, tiles, func), n0 = grp
            gn = min(GN, N - n0)
            wb = wbr.next()
            for kc in range(K // 128):
                ws = wsr.next()
                ld(g, "act", ws[:, :gn], W[kc * 128:(kc + 1) * 128, n0:n0 + gn])
                cp(g, ce(), wb[:, kc, :gn], ws[:, :gn])
            return wb
        groups = [(j, n0) for j in jobs for n0 in range(0, j[3], GN)]
        wb_next = load_w(groups[0])
        for gi, grp in enumerate(groups):
            (XT, K, W, N, YT, tiles, func), n0 = grp
            KC = K // 128
            gn = min(GN, N - n0)
            wb = wb_next
            if gi + 1 < len(groups):
                wb_next = load_w(groups[gi + 1])
            for (t0, tsz) in tiles:
                xt = xr.next()
                ld(g, "sp", xt[:, :KC, :tsz], XT[:, t0:t0 + tsz].rearrange("(kc p) t -> p kc t", p=128))
                yo = yr.next()
                for m in range(gn // 128):
                    ps = psr.next()
                    for kc in range(KC):
                        mm(g, ps[:, :tsz], wb[:, kc, m * 128:(m + 1) * 128], xt[:, kc, :tsz], kc == 0, kc == KC - 1)
                    if func == AF.Identity and ee() == "dve":
                        cp(g, "dve", yo[:, m, :tsz], ps[:, :tsz])
                    else:
                        act(g, yo[:, m, :tsz], ps[:, :tsz], func)
                st(g, "pool", YT[n0:n0 + gn, t0:t0 + tsz].rearrange("(m p) t -> p m t", p=128), yo[:, :gn // 128, :tsz])


def transpose_dram(g, src, A, B, dst, dt, ident):
    with g.stage():
        sr = g.pool_of(2, "ts", [128, B], dt)
        orr = g.pool_of(3, "to", [128, 4, 128], dt)
        psr = g.pool_of(3, "tp", [128, 4, 128], dt, kind="ps")
        ee = Alt(["act", "dve"])
        for a0 in range(0, A, 128):
            s_ = sr.next()
            ld(g, "sp", s_[:, :], src[a0:a0 + 128, :])
            for b0 in range(0, B, 512):
                nb = min(4, (B - b0) // 128)
                ps = psr.next()
                for i in range(nb):
                    tr(g, ps[:, i, :], s_[:, b0 + i * 128:b0 + (i + 1) * 128], ident)
                ot = orr.next()
                cp(g, ee(), ot[:, :nb, :], ps[:, :nb, :])
                st(g, "pool", dst[b0:b0 + nb * 128, a0:a0 + 128].rearrange("(i p) a -> p i a", p=128), ot[:, :nb, :])


def vec_layout(c):
    KD, KR, KC = c.D // 128, c.R // 128, c.CW // 128
    off, o = {}, 0

    def add(n, w):
        nonlocal o
        off[n] = o
        o += w
    add("n1g", KD); add("n2g", KD); add("fin", KD); add("bmod", 6 * KD)
    for d in range(2):
        for n in ("mu_r", "mu_k", "mu_v", "w0", "a0", "kk", "ka"):
            add(f"{n}{d}", KR)
        add(f"mu_wa{d}", 1)
    for n in ("rk", "lng", "lnb"):
        add(n, KR)
    add("convw", KC * 31); add("convb", KC); add("clg", KC); add("clb", KC)
    return off, o


def fm(a):
    n = a.shape[-1] // 128
    return np.swapaxes(a.reshape(a.shape[:-1] + (n, 128)), -1, -2)


def pack_vecs(c, I):
    off, nv = vec_layout(c)
    R = c.R
    V = np.zeros((c.NL, 128, nv), np.float32)

    def put(n, a):
        V[:, :, off[n]:off[n] + a.shape[-1]] = a
    put("n1g", fm(I["norm1_g"])); put("n2g", fm(I["norm2_g"]))
    put("fin", np.broadcast_to(fm(I["final_g"])[None], (c.NL, 128, c.D // 128)))
    bm = I["b_mod"].reshape(c.NL, 6, c.D)
    put("bmod", np.concatenate([fm(bm[:, j]) for j in range(6)], axis=-1))
    for d in range(2):
        mu = I["tok_mu"][:, d]
        put(f"mu_r{d}", fm(mu[:, 0:R])); put(f"mu_k{d}", fm(mu[:, R:2 * R])); put(f"mu_v{d}", fm(mu[:, 2 * R:3 * R]))
        put(f"mu_wa{d}", mu[:, 3 * R:3 * R + 128][:, :, None])
        put(f"w0{d}", fm(I["w0"][:, d])); put(f"a0{d}", fm(I["a0"][:, d]))
        put(f"kk{d}", fm(I["k_k"][:, d])); put(f"ka{d}", fm(I["k_a"][:, d]))
    put("rk", fm(I["r_k"])); put("lng", fm(I["lnx_g"])); put("lnb", fm(I["lnx_b"]))
    cw = np.transpose(I["conv_w"], (0, 2, 1))
    KC = c.CW // 128
    cw = cw.reshape(c.NL, KC, 128, 31).transpose(0, 2, 1, 3).reshape(c.NL, 128, KC * 31)
    put("convw", cw)
    put("convb", fm(I["conv_b"])); put("clg", fm(I["conv_ln_g"])); put("clb", fm(I["conv_ln_b"]))
    return V


def make_consts():
    C = np.zeros((128, 6, 128), np.float32)
    C[:, 0, :] = np.eye(128)
    C[:, 1, :] = 1.0
    C[:64, 2, :64] = 1.0
    C[64:, 2, 64:] = 1.0
    s = np.arange(128)[:, None] % 64
    t = np.arange(128)[None, :] % 64
    q = (np.arange(128)[None, :] < 64)
    C[:, 3, :] = np.where(q, s < t, s <= t)
    C[:, 4, :] = np.where(q, s > t, s >= t)
    return C


C0 = float(np.exp(-0.5))
WNAMES = ["w_mod", "w_in", "w2a2", "g2", "w_rwkv_out", "w_conv_out", "w_o", "w_router", "w_exp_gate", "w_exp_up", "w_exp_down"]


def build(c, debug=(), skip=()):
    nc = bass.Bass("TRN2", target_bir_lowering=False)
    g = Builder(nc)
    D, NT, R, CW, E, FF, NL, CTX, L = c.D, c.NT, c.R, c.CW, c.E, c.FF, c.NL, c.CTX, c.L
    KD, KR, KC, NH = D // 128, R // 128, CW // 128, c.NH
    NCH = NT // 64
    off, NV = vec_layout(c)

    def din(name, shape, dt=F32):
        return nc.dram_tensor(name, list(shape), dt, kind="ExternalInput").ap()

    def dscr(name, shape, dt):
        return nc.dram_tensor(name, list(shape), dt, kind=("ExternalOutput" if name in debug else "Internal")).ap()

    xT0 = din("xT0", [D, NT])
    cs_d = din("cs", [128, KD, 2])
    consts_d = din("consts", [128, 6, 128])
    vecs_d = din("vecs", [NL, 128, NV])
    Wd = dict(w_mod=din("w_mod", [NL, D, 6 * D]), w_in=din("w_in", [NL, D, c.INC]), w2a2=din("w2a2", [NL, 2, 128, R]),
              g2=din("g2", [NL, 128, R]), w_rwkv_out=din("w_rwkv_out", [NL, R, D]), w_conv_out=din("w_conv_out", [NL, CW, D]),
              w_o=din("w_o", [NL, D, D]), w_router=din("w_router", [NL, D, E]), w_exp_gate=din("w_exp_gate", [NL, E, D, FF]),
              w_exp_up=din("w_exp_up", [NL, E, D, FF]), w_exp_down=din("w_exp_down", [NL, E, FF, D]))
    outT = nc.dram_tensor("outT", [D, L], F32, kind="ExternalOutput").ap()

    xres = dscr("xres", [D, NT], F32)
    hT = dscr("hT", [D, NT], BF16)
    PT = dscr("PT", [c.INC, NT], BF16)
    QR = [dscr(f"QR{d}", [NCH, 128, KR, 2, 64], BF16) for d in range(2)]
    BK = [dscr(f"BK{d}", [NCH, 128, KR, 2, 64], BF16) for d in range(2)]
    VT = [dscr(f"VT{d}", [3, R, NT], BF16) for d in range(2)]
    VM = [dscr(f"VM{d}", [3, NT, R], BF16) for d in range(2)]
    BON = [dscr(f"BON{d}", [R, NT], F32) for d in range(2)]
    Yd = [dscr(f"Y{d}", [NT, R], F32) for d in range(2)]
    YT = [dscr(f"YT{d}", [R, NT], F32) for d in range(2)]
    GT = dscr("GT", [R, NT], BF16)
    YG = dscr("YG", [R, NT], BF16)
    BR = dscr("BR", [D, NT], BF16)
    SC = dscr("SC", [CW, NT], BF16)
    BC = dscr("BC", [D, NT], BF16)
    ZT = dscr("ZT", [D, NT], BF16)
    MT = dscr("MT", [D, NT], F32)
    h2T = dscr("h2T", [D, NT], BF16)
    H2 = dscr("H2", [NT, D], BF16)
    AFF = dscr("AFF", [E, NT], F32)
    SL = c.CAP_C + c.CAP_L
    IDX = dscr("IDX", [E, SL], U32)
    VAL = dscr("VAL", [E, SL], F32)
    XST = dscr("XST", [E, D, SL], BF16)
    GH = dscr("GH", [E, FF, SL], BF16)
    UH = dscr("UH", [E, FF, SL], BF16)
    YET = dscr("YET", [E, D, SL], F32)
    MO = dscr("MO", [NT, D], F32)
    MOT = dscr("MOT", [D, NT], F32)

    cst = g.sb("cst", [128, 6, 128], F32, stage=False)
    cstb = g.sb("cstb", [128, 6, 128], BF16, stage=False)
    vecs = g.sb("vecs", [128, NV], F32, stage=False)
    csil = g.sb("csil", [128, KD, 2], F32, stage=False)
    modT = g.sb("modT", [128, 6 * KD, 2], F32, stage=False)
    sc1 = g.sb("sc1", [128, KD, 2], F32, stage=False)
    sc2 = g.sb("sc2", [128, KD, 2], F32, stage=False)
    omka = g.sb("omka", [128, 2, KR], F32, stage=False)
    GC = g.sb("GC", [128, 2, KR, NCH], F32, stage=False)
    ld(g, "sp", cst[:, :, :], consts_d)
    cp(g, "dve", cstb[:, :, :], cst[:, :, :])
    ld(g, "sp", csil[:, :, :], cs_d)
    act(g, csil[:, :, :], csil[:, :, :], AF.Silu)
    ident, ones, bo = cst[:, 0, :], cst[:, 1, :], cst[:, 2, :]
    identb, bob = cstb[:, 0, :], cstb[:, 2, :]

    def V_(name, i=0, n=1):
        return vecs[:, off[name] + i:off[name] + i + n]

    ctx_tiles = [(t0, min(512, CTX - t0), 1) for t0 in range(0, CTX, 512)]
    lat_tiles = [(CTX + t0, 512, 0) for t0 in range(0, L, 512)]
    all_tiles = ctx_tiles + lat_tiles

    def stage_mods(l):
        with g.stage():
            ld(g, "sp", vecs[:, :], vecs_d[l])
            wr = g.pool_of(2, "mw", [128, KD, 512], F32)
            psr = g.pool_of(2, "mp", [128, 4, 2], F32, kind="ps")
            for nb in range(6 * D // 512):
                w = wr.next()
                ld(g, "sp", w[:, :, :], Wd["w_mod"][l][:, nb * 512:(nb + 1) * 512].rearrange("(kc p) n -> p kc n", p=128))
                ps = psr.next()
                for j in range(4):
                    for kc in range(KD):
                        mm(g, ps[:, j, :], w[:, kc, j * 128:(j + 1) * 128], csil[:, kc, :], kc == 0, kc == KD - 1)
                for j2 in range(2):
                    tt(g, "dve", modT[:, nb * 4:nb * 4 + 4, j2], ps[:, :, j2], V_("bmod", nb * 4, 4), ALU.add)
            for j2 in range(2):
                stt(g, "dve", sc1[:, :, j2], modT[:, KD:2 * KD, j2], 1.0, V_("n1g", 0, KD), ALU.add, ALU.mult)
                stt(g, "dve", sc2[:, :, j2], modT[:, 4 * KD:5 * KD, j2], 1.0, V_("n2g", 0, KD), ALU.add, ALU.mult)
            for d in range(2):
                ts(g, "dve", omka[:, d, :], V_(f"ka{d}", 0, KR), -1.0, ALU.mult, 1.0, ALU.add)

    def stage_norm(src, dst, scv, sh_off, tiles, router_l=None, final=False):
        with g.stage():
            xr = g.pool_of(2, "nx", [128, KD, 512], F32)
            sq = g.sb("nsq", [128, KD, 512], F32)
            rs = g.sb("nrs", [128, 512], F32)
            hr = g.pool_of(2, "nh", [128, KD, 512], F32 if final else BF16)
            ps = g.ps("nps", [128, 512])
            if router_l is not None:
                wrt = g.sb("nwr", [128, KD, E], F32)
                ld(g, "sp", wrt[:, :, :], Wd["w_router"][router_l].rearrange("(kc p) e -> p kc e", p=128))
                hf = g.sb("nhf", [128, KD, 512], F32)
                ps2 = g.ps("nps2", [E, 512])
                ps3 = g.ps("nps3", [E, 512])
                ex = g.sb("nex", [E, 512], F32)
                af = g.sb("naf", [E, 512], F32)
            for (t0, tsz, j) in tiles:
                x = xr.next()
                ld(g, "sp", x[:, :, :tsz], src[:, t0:t0 + tsz].rearrange("(kc p) t -> p kc t", p=128))
                act(g, sq[:, :, :tsz], x[:, :, :tsz], AF.Square)
                for kc in range(KD):
                    mm(g, ps[:, :tsz], ones, sq[:, kc, :tsz], kc == 0, kc == KD - 1)
                act(g, rs[:, :tsz], ps[:, :tsz], AF.Sqrt, bias=1e-6, scale=1.0 / D)
                recip(g, rs[:, :tsz], rs[:, :tsz])
                h = hr.next()
                for kc in range(KD):
                    tt(g, "dve", x[:, kc, :tsz], x[:, kc, :tsz], rs[:, :tsz], ALU.mult)
                    if final:
                        act(g, h[:, kc, :tsz], x[:, kc, :tsz], AF.Identity, scale=V_("fin", kc))
                    elif router_l is not None:
                        act(g, hf[:, kc, :tsz], x[:, kc, :tsz], AF.Identity, bias=modT[:, sh_off + kc, j:j + 1], scale=scv[:, kc, j:j + 1])
                        cp(g, "pool", h[:, kc, :tsz], hf[:, kc, :tsz])
                    else:
                        act(g, h[:, kc, :tsz], x[:, kc, :tsz], AF.Identity, bias=modT[:, sh_off + kc, j:j + 1], scale=scv[:, kc, j:j + 1])
                if final:
                    st(g, "pool", dst[:, t0 - CTX:t0 - CTX + tsz].rearrange("(kc p) t -> p kc t", p=128), h[:, :, :tsz])
                else:
                    st(g, "pool", dst[:, t0:t0 + tsz].rearrange("(kc p) t -> p kc t", p=128), h[:, :, :tsz])
                if router_l is not None:
                    for kc in range(KD):
                        mm(g, ps2[:, :tsz], wrt[:, kc, :], hf[:, kc, :tsz], kc == 0, kc == KD - 1)
                    act(g, ex[:, :tsz], ps2[:, :tsz], AF.Exp)
                    mm(g, ps3[:, :tsz], cst[0:E, 1, 0:E], ex[:, :tsz])
                    recip(g, af[:, :tsz], ps3[:, :tsz])
                    tt(g, "dve", af[:, :tsz], af[:, :tsz], ex[:, :tsz], ALU.mult)
                    st(g, "pool", AFF[:, t0:t0 + tsz], af[:, :tsz])

    def stage_prep(l, d):
        offw = c.OFF_WA_F if d == 0 else c.OFF_WA_B
        mask_unused = None
        with g.stage():
            w2a2 = g.sb("pw", [128, R], BF16)
            w2f = g.sb("pwf", [128, R], F32)
            ld(g, "sp", w2f[:, :], Wd["w2a2"][l, d])
            cp(g, "dve", w2a2[:, :], w2f[:, :])
            zb = g.pool_of(2, "pz", [128, 4, 513], BF16)
            fsets = []
            for si in range(2):
                fd = {n: g.sb(f"p{n}{si}", [128, 512], F32) for n in
                      ("r", "k", "v", "wa", "d", "sg", "a", "kk", "ss", "km", "bv", "Lc", "Le", "TB", "X3", "X4", "g1", "g2", "g3", "g4", "tmp")}
                fd["sqb"] = g.sb(f"psqb{si}", [128, 512], BF16)
                fd["tot"] = g.sb(f"ptot{si}", [128, 8], F32)
                for pn in ("psA", "psB", "psC", "psD"):
                    fd[pn] = g.ps(f"p{pn}{si}", [128, 512])
                fsets.append(fd)
            f = fsets[0]
            wab = g.sb("pwab", [128, 512], BF16)
            qro = g.pool_of(1, "pqr", [128, 8, KR, 2, 64], BF16)
            bko = g.pool_of(1, "pbk", [128, 8, KR, 2, 64], BF16)
            vo = g.pool_of(2, "pvo", [128, 3, 512], BF16)
            bono = g.pool_of(2, "pbo", [128, 512], F32)
            for (t0, tsz, j) in all_tiles:
                nch = tsz // 64
                ch0 = t0 // 64
                seq0, seq1 = (0, CTX) if j == 1 else (CTX, NT)
                def load_shift(zt, slot, row0):
                    if d == 0:
                        if t0 == seq0:
                            mset(g, "pool", zt[:, slot, 0:1], 0.0)
                            ld(g, "sp", zt[:, slot, 1:tsz + 1], PT[row0:row0 + 128, t0:t0 + tsz])
                        else:
                            ld(g, "sp", zt[:, slot, 0:tsz + 1], PT[row0:row0 + 128, t0 - 1:t0 + tsz])
                        return zt[:, slot, 1:tsz + 1], zt[:, slot, 0:tsz]
                    else:
                        if t0 + tsz == seq1:
                            mset(g, "pool", zt[:, slot, tsz:tsz + 1], 0.0)
                            ld(g, "sp", zt[:, slot, 0:tsz], PT[row0:row0 + 128, t0:t0 + tsz])
                        else:
                            ld(g, "sp", zt[:, slot, 0:tsz + 1], PT[row0:row0 + 128, t0:t0 + tsz + 1])
                        return zt[:, slot, 0:tsz], zt[:, slot, 1:tsz + 1]

                def mix(out, z, zs, mu, f=None):
                    f = f or fsets[0]
                    tt(g, "dve", f["d"][:, :tsz], zs, z, ALU.subtract)
                    stt(g, "dve", out, f["d"][:, :tsz], mu, z, ALU.mult, ALU.add)
                zt = zb.next()
                z, zs = load_shift(zt, 3, offw)
                f = fsets[0]
                mix(f["wa"][:, :tsz], z, zs, V_(f"mu_wa{d}"), f)
                act(g, wab[0:64, :tsz], f["wa"][0:64, :tsz], AF.Tanh)
                cp(g, "pool", wab[64:128, :tsz], f["wa"][64:128, :tsz])
                qo, bo_ = qro.next(), bko.next()
                def cc_body(cc):
                    f = fsets[cc % 2]
                    sqb, tot, psA, psB, psC, psD = f["sqb"], f["tot"], f["psA"], f["psB"], f["psC"], f["psD"]
                    zt = zb.next()
                    z, zs = load_shift(zt, 0, 0 * R + cc * 128); mix(f["r"][:, :tsz], z, zs, V_(f"mu_r{d}", cc), f)
                    z, zs = load_shift(zt, 1, 1 * R + cc * 128); mix(f["k"][:, :tsz], z, zs, V_(f"mu_k{d}", cc), f)
                    z, zs = load_shift(zt, 2, 2 * R + cc * 128); mix(f["v"][:, :tsz], z, zs, V_(f"mu_v{d}", cc), f)
                    yield
                    mm(g, psA[:, :tsz], w2a2[0:64, cc * 128:(cc + 1) * 128], wab[0:64, :tsz])
                    mm(g, psB[:, :tsz], w2a2[64:128, cc * 128:(cc + 1) * 128], wab[64:128, :tsz])
                    act(g, f["sg"][:, :tsz], psA[:, :tsz], AF.Sigmoid, bias=V_(f"w0{d}", cc))
                    act(g, f["a"][:, :tsz], psB[:, :tsz], AF.Sigmoid, bias=V_(f"a0{d}", cc))
                    yield
                    ts(g, "dve", f["kk"][:, :tsz], f["k"][:, :tsz], V_(f"kk{d}", cc), ALU.mult)
                    tt(g, "pool", sqb[:, :tsz], f["kk"][:, :tsz], f["kk"][:, :tsz], ALU.mult)
                    mm(g, psC[:, :tsz], bob, sqb[:, :tsz])
                    yield
                    act(g, f["ss"][:, :tsz], psC[:, :tsz], AF.Sqrt)
                    ts(g, "dve", f["ss"][:, :tsz], f["ss"][:, :tsz], 1e-12, ALU.max)
                    recip(g, f["ss"][:, :tsz], f["ss"][:, :tsz])
                    tt(g, "dve", f["kk"][:, :tsz], f["kk"][:, :tsz], f["ss"][:, :tsz], ALU.mult)
                    ts(g, "dve", f["tmp"][:, :tsz], f["a"][:, :tsz], V_(f"ka{d}", cc), ALU.mult, omka[:, d, cc:cc + 1], ALU.add)
                    tt(g, "dve", f["km"][:, :tsz], f["k"][:, :tsz], f["tmp"][:, :tsz], ALU.mult)
                    tt(g, "pool", f["bv"][:, :tsz], f["kk"][:, :tsz], f["a"][:, :tsz], ALU.mult)
                    yield
                    stt(g, "dve", sqb[:, :tsz], f["r"][:, :tsz], V_("rk", cc), f["km"][:, :tsz], ALU.mult, ALU.mult)
                    mm(g, psD[:, :tsz], bob, sqb[:, :tsz])
                    yield
                    bn = bono.next()
                    tt(g, "dve", bn[:, :tsz], psD[:, :tsz], f["v"][:, :tsz], ALU.mult)
                    st(g, "pool", BON[d][cc * 128:(cc + 1) * 128, t0:t0 + tsz], bn[:, :tsz])
                    for ci in range(nch):
                        sl = slice(ci * 64, (ci + 1) * 64)
                        g.op("dve", lambda h, sl=sl, f=f: h.tensor_tensor_scan(out=f["Lc"].ap[:, sl], data0=cst.ap[:, 1, 0:64], data1=f["sg"].ap[:, sl],
                                                                          initial=0.0, op0=ALU.mult, op1=ALU.add),
                             [f["sg"], cst], [f["Lc"]])
                    tt(g, "pool", f["Le"][:, :tsz], f["Lc"][:, :tsz], f["sg"][:, :tsz], ALU.subtract)
                    yield
                    lc3 = f["Lc"][:, :tsz].re("p (c t) -> p c t", t=64)
                    cp(g, "dve", tot[:, :nch], lc3[:, :, 63])
                    act(g, GC[:, d, cc, ch0:ch0 + nch], tot[:, :nch], AF.Exp, scale=-C0)
                    for ci in range(nch):
                        ts(g, "pool", f["TB"][:, ci * 64:(ci + 1) * 64], f["Lc"][:, ci * 64:(ci + 1) * 64], 0.0, ALU.mult, tot[:, ci:ci + 1], ALU.add)
                    tt(g, "dve", f["X3"][:, :tsz], f["TB"][:, :tsz], f["Lc"][:, :tsz], ALU.subtract)
                    tt(g, "pool", f["X4"][:, :tsz], f["TB"][:, :tsz], f["Le"][:, :tsz], ALU.subtract)
                    yield
                    if d == 0:
                        spec = (("Lc", -C0), ("Le", -C0), ("Lc", C0), ("X3", -C0))
                    else:
                        spec = (("X4", -C0), ("X3", -C0), ("X4", C0), ("Le", -C0))
                    for gi, (srcn, scl) in enumerate(spec):
                        act(g, f[f"g{gi + 1}"][:, :tsz], f[srcn][:, :tsz], AF.Exp, scale=scl)
                    yield

                    def c3(v):
                        return v.re("p (c t) -> p c t", t=64)
                    tt(g, "dve", qo[:, :nch, cc, 0, :], c3(f["kk"][:, :tsz]), c3(f["g2"][:, :tsz]), ALU.mult)
                    tt(g, "pool", qo[:, :nch, cc, 1, :], c3(f["r"][:, :tsz]), c3(f["g1"][:, :tsz]), ALU.mult)
                    tt(g, "dve", bo_[:, :nch, cc, 0, :], c3(f["bv"][:, :tsz]), c3(f["g3"][:, :tsz]), ALU.mult)
                    tt(g, "pool", bo_[:, :nch, cc, 1, :], c3(f["km"][:, :tsz]), c3(f["g3"][:, :tsz]), ALU.mult)
                    vv = vo.next()
                    cp(g, "act", vv[:, 0, :tsz], f["v"][:, :tsz])
                    tt(g, "dve", vv[:, 1, :tsz], f["bv"][:, :tsz], f["g4"][:, :tsz], ALU.mult)
                    tt(g, "pool", vv[:, 2, :tsz], f["km"][:, :tsz], f["g4"][:, :tsz], ALU.mult)
                    st(g, "pool", VT[d][:, cc * 128:(cc + 1) * 128, t0:t0 + tsz].rearrange("q p t -> p q t"), vv[:, :, :tsz])
                for cc0 in range(0, KR, 2):
                    gens = [cc_body(cc_) for cc_ in range(cc0, min(KR, cc0 + 2))]
                    while gens:
                        for gn_ in list(gens):
                            try:
                                next(gn_)
                            except StopIteration:
                                gens.remove(gn_)
                st(g, "act", QR[d][ch0:ch0 + nch].rearrange("c p k w t -> p c (k w t)"), qo[:, :nch].re("p c k w t -> p c (k w t)"))
                st(g, "act", BK[d][ch0:ch0 + nch].rearrange("c p k w t -> p c (k w t)"), bo_[:, :nch].re("p c k w t -> p c (k w t)"))
        for q in range(3):
            transpose_dram(g, VT[d][q], R, NT, VM[d][q], BF16, identb)

    def stage_scan2():
        IDT = BF16
        HH = max(1, NH // 2)
        halves = [list(range(h0, min(NH, h0 + HH))) for h0 in range(0, NH, HH)]
        with g.stage():
            S = []
            for d in range(2):
                r = {}
                r["Hs"] = g.sb(f"sH{d}", [128, KR, 64], F32)
                r["Hb2"] = g.sb(f"sHb2{d}", [128, NH, 64], BF16)
                mset(g, "dve", r["Hs"][:, :, :], 0.0)
                mset(g, "pool", r["Hb2"][:, :, :], 0.0)
                r["qrr"] = g.pool_of(2, f"sqr{d}", [128, KR, 2, 64], BF16)
                r["bkr"] = g.pool_of(2, f"sbk{d}", [128, KR, 2, 64], BF16)
                r["uvr"] = g.pool_of(2, f"suv{d}", [128, R], BF16)
                r["bpr"] = g.pool_of(2, f"sbp{d}", [128, R], BF16)
                zvs = [g.sb(f"szv{d}{i}", [128, R], BF16) for i in range(2)]
                for zv_ in zvs:
                    mset(g, "pool", zv_[:, :], 0.0)
                r["zvr"] = Ring(zvs)
                r["Gs"] = g.sb(f"sG{d}", [128, NH, 128], BF16)
                r["P"] = [g.sb(f"sP{d}{i}", [64, NH, 2, 64], IDT) for i in range(2)]
                r["Pt"] = [g.sb(f"sPt{d}{i}", [64, NH, 64], IDT) for i in range(2)]
                r["X"] = [g.sb(f"sX{d}{i}", [64, NH, 64], F32) for i in range(2)]
                r["Xb"] = g.sb(f"sXb{d}", [64, NH, 64], IDT)
                r["Wsb"] = g.sb(f"sW{d}", [64, NH, 64], BF16)
                r["ysr"] = g.pool_of(2, f"sy{d}", [64, R], F32)
                r["psG"] = g.ps(f"spG{d}", [128, HH, 128])
                r["psB"] = g.ps(f"spB{d}", [64, HH, 64])
                r["psC"] = g.ps(f"spC{d}", [64, HH, 64])
                S.append(r)
            eyeI = g.sb("seye", [64, NH, 64], F32)
            for h_ in range(NH):
                cp(g, "pool", eyeI[:, h_, :], cst[0:64, 0, 0:64])

            def chunk_gen(d, ch):
                r = S[d]
                maskG = cst[:, 3 + d, :]
                maskA = cst[0:64, 4 - d, 0:64]
                Hs, Hb2, Gs, P_, Pt, X_, Xb, Wsb = r["Hs"], r["Hb2"], r["Gs"], r["P"], r["Pt"], r["X"], r["Xb"], r["Wsb"]
                psG, psB, psC = r["psG"], r["psB"], r["psC"]
                t0 = ch * 64
                qr, bk, uv, bp, zv = r["qrr"].next(), r["bkr"].next(), r["uvr"].next(), r["bpr"].next(), r["zvr"].next()
                ld(g, "sp", qr[:, :, :, :], QR[d][ch])
                ld(g, "sp", bk[:, :, :, :], BK[d][ch])
                ld(g, "sp", uv[64:128, :], VM[d][0][t0:t0 + 64, :])
                ld(g, "sp", zv[64:128, :], VM[d][0][t0:t0 + 64, :])
                ld(g, "sp", bp[0:64, :], VM[d][1][t0:t0 + 64, :])
                ld(g, "sp", bp[64:128, :], VM[d][2][t0:t0 + 64, :])
                yield
                for hs in halves:
                    h0, n = hs[0], len(hs)
                    hsl = slice(h0, h0 + n)
                    for i, h_ in enumerate(hs):
                        p0, cc = (h_ % 2) * 64, h_ // 2
                        mm(g, psG[:, i, :], bk[p0:p0 + 64, cc, :, :].re("p w t -> p (w t)"), qr[p0:p0 + 64, cc, :, :].re("p w t -> p (w t)"))
                        mm(g, psB[:, i, :], qr[p0:p0 + 64, cc, 0, :], bk[p0:p0 + 64, cc, 0, :])
                    tt(g, "dve", Gs[:, hsl, :], psG[:, :n, :], maskG.bc3(n), ALU.mult)
                    stt(g, "dve", Pt[0][:, hsl, :], psB[:, :n, :], -1.0, maskA.bc3(n), ALU.mult, ALU.mult)
                    act(g, P_[0][:, hsl, 0, :], Gs[0:64, hsl, 0:64], AF.Identity, scale=-1.0)
                    tt(g, "dve", X_[0][:, hsl, :], eyeI[:, hsl, :], P_[0][:, hsl, 0, :], ALU.add)
                    yield
                for hs in halves:
                    h0, n = hs[0], len(hs)
                    hsl = slice(h0, h0 + n)
                    for i, h_ in enumerate(hs):
                        mm(g, psC[:, i, :], P_[0][:, h_, 0, :], Pt[0][:, h_, :])
                        mm(g, psB[:, i, :], Pt[0][:, h_, :], P_[0][:, h_, 0, :])
                    cp(g, "act", Pt[1][:, hsl, :], psC[:, :n, :])
                    cp(g, "dve", P_[1][:, hsl, 0, :], psB[:, :n, :])
                    cp(g, "act", P_[1][:, hsl, 1, :], X_[0][:, hsl, :])
                    yield
                cur, xc = 1, 0
                for jj in range(1, 6):
                    nxt, xn = 1 - cur, 1 - xc
                    for hs in halves:
                        h0, n = hs[0], len(hs)
                        hsl = slice(h0, h0 + n)
                        for i, h_ in enumerate(hs):
                            if jj < 5:
                                mm(g, psG[0:64, i, :], Pt[cur][:, h_, :], P_[cur][:, h_, :, :].re("p w t -> p (w t)"))
                                mm(g, psC[:, i, :], P_[cur][:, h_, 0, :], Pt[cur][:, h_, :])
                            else:
                                mm(g, psG[0:64, i, 64:128], Pt[cur][:, h_, :], P_[cur][:, h_, 1, :])
                        if jj < 5:
                            cp(g, "act", Pt[nxt][:, hsl, :], psC[:, :n, :])
                            cp(g, "dve", P_[nxt][:, hsl, 0, :], psG[0:64, :n, 0:64])
                        tt(g, "dve", X_[xn][:, hsl, :], X_[xc][:, hsl, :], psG[0:64, :n, 64:128], ALU.add)
                        if jj < 5:
                            cp(g, "act", P_[nxt][:, hsl, 1, :], X_[xn][:, hsl, :])
                        else:
                            cp(g, "act", Xb[:, hsl, :], X_[xn][:, hsl, :])
                        yield
                    cur, xc = nxt, xn
                ys = r["ysr"].next()
                for hs in halves:
                    h0, n = hs[0], len(hs)
                    hsl = slice(h0, h0 + n)
                    csl = slice(h0 * 64, (h0 + n) * 64)
                    for i, h_ in enumerate(hs):
                        cc = h_ // 2
                        mm(g, psB[:, i, :], qr[:, cc, 0, :], Hb2[:, h_, :], True, False)
                        mm(g, psB[:, i, :], Gs[:, h_, 0:64], zv[:, h_ * 64:(h_ + 1) * 64], False, True)
                    cp(g, "act", Wsb[:, hsl, :], psB[:, :n, :])
                    yield
                    for i, h_ in enumerate(hs):
                        mm(g, psC[:, i, :], Xb[:, h_, :], Wsb[:, h_, :])
                    act(g, uv[0:64, csl].re("p (h v) -> p h v", v=64), psC[:, :n, :], AF.Identity, scale=-1.0)
                    yield
                    for i, h_ in enumerate(hs):
                        cc = h_ // 2
                        mm(g, psB[:, i, :], qr[:, cc, 1, :], Hb2[:, h_, :], True, False)
                        mm(g, psB[:, i, :], Gs[:, h_, 64:128], uv[:, h_ * 64:(h_ + 1) * 64], False, True)
                    cp(g, "dve", ys[:, csl].re("p (h v) -> p h v", v=64), psB[:, :n, :])
                    for i, h_ in enumerate(hs):
                        cc = h_ // 2
                        mm(g, psG[:, i, 0:64], bp[:, cc * 128:(cc + 1) * 128], uv[:, h_ * 64:(h_ + 1) * 64])
                    yield
                    for i, h_ in enumerate(hs):
                        par, cc = h_ % 2, h_ // 2
                        pr = slice(par * 64, par * 64 + 64)
                        stt(g, "dve", Hs[pr, cc, :], Hs[pr, cc, :], GC[pr, d, cc, ch:ch + 1], psG[pr, i, 0:64], ALU.mult, ALU.add)
                    yield
                st(g, "pool", Yd[d][t0:t0 + 64, :], ys[:, :])
                cp(g, "act", Hb2[0:64, 0::2, :], Hs[0:64, :, :])
                if NH > 1:
                    cp(g, "pool", Hb2[64:128, 1::2, :], Hs[64:128, :, :])

            ctx_ch = list(range(CTX // 64))
            lat_ch = list(range(CTX // 64, NCH))
            orders = [ctx_ch + lat_ch, ctx_ch[::-1] + lat_ch[::-1]]
            for i in range(NCH):
                gens = [chunk_gen(0, orders[0][i]), chunk_gen(1, orders[1][i])]
                while gens:
                    for gn in list(gens):
                        try:
                            next(gn)
                        except StopIteration:
                            gens.remove(gn)
        for d in range(2):
            transpose_dram(g, Yd[d], NT, R, YT[d], F32, ident)

    def stage_post(tiles):
        with g.stage():
            ya = g.pool_of(2, "oa", [128, 512], F32); yb = g.pool_of(2, "ob", [128, 512], F32)
            b0 = g.pool_of(2, "o0", [128, 512], F32); b1 = g.pool_of(2, "o1", [128, 512], F32)
            gt = g.pool_of(2, "og", [128, 512], BF16)
            dd = g.sb("od", [128, 512], F32); sq = g.sb("osq", [128, 512], F32); rs = g.sb("ors", [128, 512], F32)
            out = g.pool_of(2, "oo", [128, 512], BF16)
            ps1 = g.ps("op1", [128, 512]); ps2 = g.ps("op2", [128, 512])
            for (t0, tsz, j) in tiles:
                for cc in range(KR):
                    rows = slice(cc * 128, (cc + 1) * 128)
                    a, b, c0_, c1_, gg = ya.next(), yb.next(), b0.next(), b1.next(), gt.next()
                    ld(g, "sp", a[:, :tsz], YT[0][rows, t0:t0 + tsz]); ld(g, "sp", b[:, :tsz], YT[1][rows, t0:t0 + tsz])
                    ld(g, "sp", c0_[:, :tsz], BON[0][rows, t0:t0 + tsz]); ld(g, "sp", c1_[:, :tsz], BON[1][rows, t0:t0 + tsz])
                    ld(g, "sp", gg[:, :tsz], GT[rows, t0:t0 + tsz])
                    tt(g, "dve", a[:, :tsz], a[:, :tsz], b[:, :tsz], ALU.add)
                    mm(g, ps1[:, :tsz], bo, a[:, :tsz])
                    stt(g, "dve", dd[:, :tsz], ps1[:, :tsz], -1.0 / 64, a[:, :tsz], ALU.mult, ALU.add)
                    act(g, sq[:, :tsz], dd[:, :tsz], AF.Square)
                    mm(g, ps2[:, :tsz], bo, sq[:, :tsz])
                    act(g, rs[:, :tsz], ps2[:, :tsz], AF.Sqrt, bias=64e-5, scale=1.0 / 64)
                    recip(g, rs[:, :tsz], rs[:, :tsz])
                    tt(g, "dve", dd[:, :tsz], dd[:, :tsz], rs[:, :tsz], ALU.mult)
                    ts(g, "dve", dd[:, :tsz], dd[:, :tsz], V_("lng", cc), ALU.mult, V_("lnb", cc), ALU.add)
                    tt(g, "pool", c0_[:, :tsz], c0_[:, :tsz], c1_[:, :tsz], ALU.add)
                    tt(g, "dve", dd[:, :tsz], dd[:, :tsz], c0_[:, :tsz], ALU.add)
                    o = out.next()
                    tt(g, "dve", o[:, :tsz], dd[:, :tsz], gg[:, :tsz], ALU.mult)
                    st(g, "pool", YG[rows, t0:t0 + tsz], o[:, :tsz])

    def stage_conf(tiles):
        with g.stage():
            ua = g.pool_of(2, "ca", [128, 512], BF16); ub = g.pool_of(2, "cb", [128, 512], BF16)
            u = g.sb("cu", [128, 512], F32)
            acc = g.sb("cacc", [128, KC, 512], F32)
            accbr = g.pool_of(2, "caccb", [128, 512], F32)
            tmr = g.pool_of(3, "ctm", [128, 512], F32)
            sq = g.sb("csq", [128, 512], F32); mean = g.sb("cmean", [128, 512], F32); rs = g.sb("crs", [128, 512], F32)
            out = g.pool_of(2, "co", [128, KC, 512], BF16)
            ps1 = g.ps("cp1", [128, 512]); ps2 = g.ps("cp2", [128, 512])
            for (t0, tsz, j) in tiles:
                rl = CTX if j == 1 else c.GW
                nr = tsz // rl
                for cc in range(KC):
                    a, b = ua.next(), ub.next()
                    ld(g, "sp", a[:, :tsz], PT[c.OFF_GLU + cc * 128:c.OFF_GLU + (cc + 1) * 128, t0:t0 + tsz])
                    ld(g, "sp", b[:, :tsz], PT[c.OFF_GLU + CW + cc * 128:c.OFF_GLU + CW + (cc + 1) * 128, t0:t0 + tsz])
                    tt(g, "pool", u[:, :tsz], a[:, :tsz], b[:, :tsz], ALU.mult)
                    ts(g, "dve", acc[:, cc, :tsz], u[:, :tsz], V_("convw", cc * 31 + 15), ALU.mult, V_("convb", cc), ALU.add)
                    u3 = u[:, :tsz].re("p (r t) -> p r t", t=rl)
                    a3 = acc[:, cc, :tsz].re("p (r t) -> p r t", t=rl)
                    accb = accbr.next()
                    mset(g, "pool", accb[:, :tsz], 0.0)
                    b3 = accb[:, :tsz].re("p (r t) -> p r t", t=rl)
                    for k in range(31):
                        s_ = k - 15
                        if s_ == 0 or abs(s_) >= rl:
                            continue
                        lo, hi = max(0, -s_), rl - max(0, s_)
                        if k < 15:
                            stt(g, "dve", a3[:, :, lo:hi], u3[:, :, lo + s_:hi + s_], V_("convw", cc * 31 + k), a3[:, :, lo:hi], ALU.mult, ALU.add)
                        else:
                            tm = tmr.next()
                            t3 = tm[:, :tsz].re("p (r t) -> p r t", t=rl)
                            act(g, t3[:, :, lo:hi], u3[:, :, lo + s_:hi + s_], AF.Identity, scale=V_("convw", cc * 31 + k))
                            tt(g, "pool", b3[:, :, lo:hi], b3[:, :, lo:hi], t3[:, :, lo:hi], ALU.add)
                    tt(g, "dve", acc[:, cc, :tsz], acc[:, cc, :tsz], accb[:, :tsz], ALU.add)
                for cc in range(KC):
                    mm(g, ps1[:, :tsz], ones, acc[:, cc, :tsz], cc == 0, cc == KC - 1)
                ts(g, "dve", mean[:, :tsz], ps1[:, :tsz], 1.0 / CW, ALU.mult)
                for cc in range(KC):
                    tt(g, "dve", acc[:, cc, :tsz], acc[:, cc, :tsz], mean[:, :tsz], ALU.subtract)
                    act(g, sq[:, :tsz], acc[:, cc, :tsz], AF.Square)
                    mm(g, ps2[:, :tsz], ones, sq[:, :tsz], cc == 0, cc == KC - 1)
                act(g, rs[:, :tsz], ps2[:, :tsz], AF.Sqrt, bias=1e-5, scale=1.0 / CW)
                recip(g, rs[:, :tsz], rs[:, :tsz])
                o = out.next()
                for cc in range(KC):
                    tt(g, "dve", acc[:, cc, :tsz], acc[:, cc, :tsz], rs[:, :tsz], ALU.mult)
                    ts(g, "pool", acc[:, cc, :tsz], acc[:, cc, :tsz], V_("clg", cc), ALU.mult, V_("clb", cc), ALU.add)
                    act(g, o[:, cc, :tsz], acc[:, cc, :tsz], AF.Silu)
                st(g, "pool", SC[:, t0:t0 + tsz].rearrange("(kc p) t -> p kc t", p=128), o[:, :, :tsz])

    def stage_merge(tiles):
        with g.stage():
            r4 = [g.pool_of(2, f"m{i}", [128, 512], BF16) for i in range(4)]
            t1 = g.sb("mt1", [128, 512], F32); t2 = g.sb("mt2", [128, 512], F32)
            out = g.pool_of(2, "mo", [128, 512], BF16)
            for (t0, tsz, j) in tiles:
                for kc in range(KD):
                    rows = slice(kc * 128, (kc + 1) * 128)
                    gr, gc_, br, bc_ = [r.next() for r in r4]
                    ld(g, "sp", gr[:, :tsz], PT[c.OFF_GATE + kc * 128:c.OFF_GATE + (kc + 1) * 128, t0:t0 + tsz])
                    ld(g, "sp", gc_[:, :tsz], PT[c.OFF_GATE + D + kc * 128:c.OFF_GATE + D + (kc + 1) * 128, t0:t0 + tsz])
                    ld(g, "sp", br[:, :tsz], BR[rows, t0:t0 + tsz]); ld(g, "sp", bc_[:, :tsz], BC[rows, t0:t0 + tsz])
                    tt(g, "dve", t1[:, :tsz], gr[:, :tsz], br[:, :tsz], ALU.mult)
                    tt(g, "pool", t2[:, :tsz], gc_[:, :tsz], bc_[:, :tsz], ALU.mult)
                    o = out.next()
                    tt(g, "dve", o[:, :tsz], t1[:, :tsz], t2[:, :tsz], ALU.add)
                    st(g, "pool", ZT[rows, t0:t0 + tsz], o[:, :tsz])

    def stage_resid(src, upd, gate_off, tiles):
        with g.stage():
            xa = g.pool_of(2, "ra", [128, KD, 512], F32); xb = g.pool_of(2, "rb", [128, KD, 512], F32)
            for (t0, tsz, j) in tiles:
                a, b = xa.next(), xb.next()
                ld(g, "sp", a[:, :, :tsz], src[:, t0:t0 + tsz].rearrange("(kc p) t -> p kc t", p=128))
                ld(g, "sp", b[:, :, :tsz], upd[:, t0:t0 + tsz].rearrange("(kc p) t -> p kc t", p=128))
                for kc in range(KD):
                    stt(g, "dve", a[:, kc, :tsz], b[:, kc, :tsz], modT[:, gate_off + kc, j:j + 1], a[:, kc, :tsz], ALU.mult, ALU.add)
                st(g, "pool", xres[:, t0:t0 + tsz].rearrange("(kc p) t -> p kc t", p=128), a[:, :, :tsz])

    def stage_moe(l, with_ctx):
        sets = ([(0, CTX, c.CAP_C, 0)] if with_ctx else []) + [(CTX, L, c.CAP_L, c.CAP_C)]
        transpose_dram(g, h2T, D, NT, H2, BF16, identb)
        with g.stage():
            aff = g.sb("ea", [E, max(L, CTX)], F32)
            vals = g.sb("ev", [E, SL], F32)
            idx = g.sb("ei", [E, SL], U32)
            z = g.sb("ez", [128, D], F32)
            mset(g, "dve", z[:, :], 0.0)
            for r0 in range(0, NT, 128):
                st(g, "sp", MO[r0:r0 + 128, :], z[:, :])
            for (s0, n, cap, so) in sets:
                ld(g, "sp", aff[:, :n], AFF[:, s0:s0 + n])
                for it in range(cap // 8):
                    v8 = vals[:, so + it * 8:so + it * 8 + 8]
                    g.op("dve", lambda h, v8=v8, n=n: h.max(out=v8.ap, in_=aff.ap[:, :n]), [aff], [vals])
                    g.op("dve", lambda h, v8=v8, n=n, it=it, so=so: h.max_index(out=idx.ap[:, so + it * 8:so + it * 8 + 8], in_max=v8.ap, in_values=aff.ap[:, :n]), [aff, vals], [idx])
                    g.op("dve", lambda h, v8=v8, n=n: h.match_replace(out=aff.ap[:, :n], in_to_replace=v8.ap, in_values=aff.ap[:, :n], imm_value=-1.0), [aff, vals], [aff])
            st(g, "sp", IDX[:, :], idx[:, :])
            st(g, "sp", VAL[:, :], vals[:, :])
        with g.stage():
            icr = g.pool_of(3, "gi", [128, 1], U32)
            xsr = g.pool_of(2, "gx", [128, D], BF16)
            xtr = g.pool_of(2, "gt", [128, KD, 128], BF16)
            psr = g.pool_of(2, "gp", [128, 4, 128], BF16, kind="ps")
            for e in range(E):
                for (s0, n, cap, so) in sets:
                    for b0 in range(0, cap, 128):
                        nb = min(128, cap - b0)
                        ic, xs, xt = icr.next(), xsr.next(), xtr.next()
                        ld(g, "sp", ic[:nb, :], IDX[e, so + b0:so + b0 + nb].rearrange("(p o) -> p o", o=1))
                        src_ap = H2; eoff = s0 * D
                        g.dma("pool", None, None, reads=[ic], writes=[xs],
                              indirect=lambda h, xs=xs, ic=ic, nb=nb, src_ap=src_ap, eoff=eoff: h.indirect_dma_start(
                                  out=xs.ap[:nb, :], out_offset=None, in_=src_ap, in_offset=bass.IndirectOffsetOnAxis(ap=ic.ap[:nb, :], axis=0), element_offset=eoff))
                        for k0 in range(0, KD, 4):
                            ps = psr.next()
                            n4 = min(4, KD - k0)
                            for i in range(n4):
                                tr(g, ps[:, i, :nb], xs[:nb, (k0 + i) * 128:(k0 + i + 1) * 128], identb[:nb, :nb])
                            cp(g, "dve" if (k0 // 4) % 2 else "act", xt[:, k0:k0 + n4, :nb], ps[:, :n4, :nb])
                        st(g, "act", XST[e][:, so + b0:so + b0 + nb].rearrange("(kc p) t -> p kc t", p=128), xt[:, :, :nb])
        sl_tiles = [(so_, min(512, cap - t_)) for (s0, n, cap, so) in sets for t_ in range(0, cap, 512) for so_ in [so + t_]]
        lock = Tile(None, "molock")
        jobs = []
        for e in range(E):
            jobs.append((XST[e], D, Wd["w_exp_gate"][l, e], FF, GH[e], sl_tiles, AF.Silu))
            jobs.append((XST[e], D, Wd["w_exp_up"][l, e], FF, UH[e], sl_tiles, AF.Identity))
        linear_fm_multi(g, jobs, BF16, GN=1024)
        with g.stage():
            gar = g.pool_of(2, "fa", [128, FF // 128, SL], BF16); gbr = g.pool_of(2, "fb", [128, FF // 128, SL], BF16)
            for e in range(E):
                ga, gb = gar.next(), gbr.next()
                ld(g, "sp", ga[:, :, :], GH[e].rearrange("(k p) t -> p k t", p=128)); ld(g, "act", gb[:, :, :], UH[e].rearrange("(k p) t -> p k t", p=128))
                tt(g, "dve" if e % 2 else "pool", ga[:, :, :], ga[:, :, :], gb[:, :, :], ALU.mult)
                st(g, "sp", GH[e].rearrange("(k p) t -> p k t", p=128), ga[:, :, :])
        linear_fm_multi(g, [(GH[e], FF, Wd["w_exp_down"][l, e], D, YET[e], sl_tiles, AF.Identity) for e in range(E)], F32, GN=1024)
        with g.stage():
            icr = g.pool_of(3, "si", [128, 1], U32); vcr = g.pool_of(3, "sv", [128, 1], F32)
            ytr = g.pool_of(2, "sy", [128, KD, 128], F32)
            yor = g.pool_of(3, "so", [128, D], F32)
            psr = g.pool_of(2, "sp", [128, 4, 128], F32, kind="ps")
            for e in range(E):
                for (s0, n, cap, so) in sets:
                    for b0 in range(0, cap, 128):
                        nb = min(128, cap - b0)
                        ic, vc, yt, yo = icr.next(), vcr.next(), ytr.next(), yor.next()
                        ld(g, "sp", ic[:nb, :], IDX[e, so + b0:so + b0 + nb].rearrange("(p o) -> p o", o=1))
                        ld(g, "sp", vc[:nb, :], VAL[e, so + b0:so + b0 + nb].rearrange("(p o) -> p o", o=1))
                        ld(g, "sp", yt[:, :, :nb], YET[e][:, so + b0:so + b0 + nb].rearrange("(kc p) t -> p kc t", p=128))
                        for k0 in range(0, KD, 4):
                            ps = psr.next()
                            n4 = min(4, KD - k0)
                            for i in range(n4):
                                tr(g, ps[:nb, i, :], yt[:, k0 + i, :nb], ident)
                            ts(g, "dve", yo[:nb, k0 * 128:(k0 + n4) * 128].re("p (i f) -> p i f", f=128), ps[:nb, :n4, :], vc[:nb, :], ALU.mult)
                        dst_ap = MO; eoff = s0 * D
                        g.dma("pool", None, None, reads=[ic, yo], writes=[lock],
                              indirect=lambda h, yo=yo, ic=ic, nb=nb, dst_ap=dst_ap, eoff=eoff: h.indirect_dma_start(
                                  out=dst_ap, out_offset=bass.IndirectOffsetOnAxis(ap=ic.ap[:nb, :], axis=0), in_=yo.ap[:nb, :], in_offset=None,
                                  compute_op=ALU.add, element_offset=eoff))
        transpose_dram(g, MO, NT, D, MOT, F32, ident)

    tiles2 = [(t0, tsz) for (t0, tsz, j) in all_tiles]
    for l in range(NL):
        last = l == NL - 1
        post_tiles = lat_tiles if last else all_tiles
        post2 = [(t0, tsz) for (t0, tsz, j) in post_tiles]
        stage_mods(l)
        stage_norm(xT0 if l == 0 else xres, hT, sc1, 0, all_tiles)
        W = Wd["w_in"][l]
        linear_fm(g, hT, D, W[:, 0:c.OFF_GLAT], c.OFF_GLAT, PT[0:c.OFF_GLAT], BF16, tiles2, GN=1024)
        linear_fm(g, hT, D, W[:, c.OFF_GLAT:c.OFF_GLU], c.GL, PT[c.OFF_GLAT:c.OFF_GLU], BF16, post2, func=AF.Sigmoid)
        linear_fm(g, hT, D, W[:, c.OFF_GLU:c.OFF_GLU + CW], CW, PT[c.OFF_GLU:c.OFF_GLU + CW], BF16, post2)
        linear_fm(g, hT, D, W[:, c.OFF_GLU + CW:c.INC], CW + 2 * D, PT[c.OFF_GLU + CW:c.INC], BF16, post2, func=AF.Sigmoid, GN=1024)
        for d in range(2):
            if "prep" not in skip:
                stage_prep(l, d)
        if "scan" not in skip:
            stage_scan2()
        linear_fm(g, PT[c.OFF_GLAT:c.OFF_GLU], c.GL, Wd["g2"][l], R, GT, BF16, post2)
        stage_post(post_tiles)
        linear_fm(g, YG, R, Wd["w_rwkv_out"][l], D, BR, BF16, post2)
        stage_conf(post_tiles)
        linear_fm(g, SC, CW, Wd["w_conv_out"][l], D, BC, BF16, post2)
        stage_merge(post_tiles)
        linear_fm(g, ZT, D, Wd["w_o"][l], D, MT, F32, post2)
        stage_resid(xT0 if l == 0 else xres, MT, 2 * KD, post_tiles)
        stage_norm(xres, h2T, sc2, 3 * KD, post_tiles, router_l=l)
        if "moe" not in skip:
            stage_moe(l, not last)
            stage_resid(xres, MOT, 5 * KD, post_tiles)
    stage_norm(xres, outT, None, 0, lat_tiles, final=True)
    g.finish()
    return nc


def prep_inputs(c, I, b):
    xT0 = np.ascontiguousarray(np.concatenate([I["ctx"][b].T, I["x"][b].T], axis=1))
    cs = np.ascontiguousarray(np.stack([fm(I["c"][b]), fm(I["c_ctx"])], axis=-1))
    m = dict(xT0=xT0, cs=cs, consts=make_consts(), vecs=pack_vecs(c, I))
    m["w2a2"] = np.ascontiguousarray(np.concatenate([I["w2"], I["a2"]], axis=2))
    for n in WNAMES:
        if n != "w2a2":
            m[n] = np.asarray(I[n])
    return m


_NC_CACHE = {}


def kernel(**inputs):
    I = {k: np.asarray(v) for k, v in inputs.items()}
    B, L, D = I["x"].shape
    c = Cfg(D=D, L=L, CTX=I["ctx"].shape[1], R=I["w_rwkv_out"].shape[1], CW=I["w_conv_out"].shape[1],
            E=I["w_router"].shape[2], FF=I["w_exp_gate"].shape[3], NL=I["w_in"].shape[0])
    nc = build(c)
    in_maps = [prep_inputs(c, I, b) for b in range(B)]
    res = run_bass_kernel_spmd(nc, in_maps, core_ids=list(range(B)))
    return np.stack([np.ascontiguousarray(res.results[b]["outT"].T) for b in range(B)], axis=0).astype(np.float32)
```
